# Optimizing a Trainium2 kernel written in Bass

```python
import math
import jax, jax.numpy as jnp
from jax import lax
import numpy as np

D_MODEL = 1024
BATCH = 16
SEQ = 2048
DEPTH = 1

N_META = 16
D_MIX = 2 * D_MODEL
SSD_HEAD_DIM = 64
SSD_HEADS = 24
D_SSD = SSD_HEADS * SSD_HEAD_DIM
SSD_GROUPS = 4
SSD_STATE = 128
SSD_CONV = 4
SSD_CHUNK = 128
D_XBC = D_SSD + 2 * SSD_GROUPS * SSD_STATE
D_SC = D_MIX - D_SSD
SC_CONV = 3
D_IN = D_SSD + D_XBC + SSD_HEADS + 3 * D_SC
PEER_HEADS = 8
PEER_KEYS = 128
PEER_EXPERTS = PEER_KEYS * PEER_KEYS
PEER_TOPK = 16
PEER_QDIM = 256
PEER_HALF = PEER_QDIM // 2
PEER_BLOCK = 512
EPS = 1e-6

kernel_name = "hymba_ssd_shortconv_peer_layer"


def rmsnorm(x, g):
    xf = x.astype(jnp.float32)
    xf = xf * lax.rsqrt(jnp.mean(xf * xf, axis=-1, keepdims=True) + EPS)
    return (xf * g.astype(jnp.float32)).astype(x.dtype)


def causal_dwconv(x, w):
    width, ch = w.shape
    return lax.conv_general_dilated(
        x, w[:, None, :].astype(x.dtype), window_strides=(1,), padding=[(width - 1, 0)],
        dimension_numbers=('NWC', 'WIO', 'NWC'), feature_group_count=ch)


def segsum(a):
    t = a.shape[-1]
    ar = jnp.broadcast_to(a[..., None], a.shape + (t,))
    ar = jnp.where(jnp.tril(jnp.ones((t, t), bool), -1), ar, 0.0)
    cs = jnp.cumsum(ar, axis=-2)
    return jnp.where(jnp.tril(jnp.ones((t, t), bool), 0), cs, -jnp.inf)


def ssd_scan(x, dt, A, Bm, Cm):
    b, l, h, p = x.shape
    g, n = Bm.shape[2], Bm.shape[3]
    r, q = h // g, SSD_CHUNK
    c = l // q
    xc = x.reshape(b, c, q, g, r, p)
    Bc = Bm.reshape(b, c, q, g, n)
    Cc = Cm.reshape(b, c, q, g, n)
    dtc = dt.reshape(b, c, q, g, r)
    a = (dtc * A.reshape(g, r)).transpose(0, 3, 4, 1, 2)
    x_dt = xc * dtc[..., None]
    a_cs = jnp.cumsum(a, axis=-1)
    decay_in = jnp.exp(segsum(a))
    cb = jnp.einsum('bclgn,bcsgn->bgcls', Cc, Bc)
    y_diag = jnp.einsum('bgcls,bgrcls,bcsgrp->bclgrp', cb, decay_in, x_dt)
    decay_states = jnp.exp(a_cs[..., -1:] - a_cs)
    states = jnp.einsum('bcsgn,bgrcs,bcsgrp->bcgrpn', Bc, decay_states, x_dt)
    states = jnp.concatenate([jnp.zeros_like(states[:, :1]), states], axis=1)
    chunk_a = jnp.pad(a_cs[..., -1], ((0, 0), (0, 0), (0, 0), (1, 0)))
    decay_chunk = jnp.exp(segsum(chunk_a))
    new_states = jnp.einsum('bgrzc,bcgrpn->bzgrpn', decay_chunk, states)
    prev_states = new_states[:, :-1]
    y_off = jnp.einsum('bclgn,bcgrpn,bgrcl->bclgrp', Cc, prev_states, jnp.exp(a_cs))
    return (y_diag + y_off).reshape(b, l, h, p)


def hybrid_mixer(n, w_in, conv_ssd_w, conv_ssd_b, dt_bias, a_log, d_skip, ssd_norm_w,
                 conv_sc_w, w_out):
    bsz, L, _ = n.shape
    proj = n @ w_in.astype(n.dtype)
    s1 = D_SSD
    s2 = s1 + D_XBC
    s3 = s2 + SSD_HEADS
    s4 = s3 + D_SC
    s5 = s4 + D_SC
    z, xbc, dt_raw, sc_b, sc_c, sc_x = jnp.split(proj, [s1, s2, s3, s4, s5], axis=-1)

    xbc = jax.nn.silu(causal_dwconv(xbc, conv_ssd_w) + conv_ssd_b.astype(n.dtype))
    xs, Bm, Cm = jnp.split(xbc, [D_SSD, D_SSD + SSD_GROUPS * SSD_STATE], axis=-1)
    xs_h = xs.reshape(bsz, L, SSD_HEADS, SSD_HEAD_DIM)
    dt = jax.nn.softplus(dt_raw.astype(jnp.float32) + dt_bias.astype(jnp.float32))
    A = -jnp.exp(a_log.astype(jnp.float32))
    pad_len = SSD_CHUNK - N_META

    def lpad(t):
        return jnp.pad(t, ((0, 0), (pad_len, 0)) + ((0, 0),) * (t.ndim - 2))

    y = ssd_scan(lpad(xs_h.astype(jnp.float32)), lpad(dt), A,
                 lpad(Bm.reshape(bsz, L, SSD_GROUPS, SSD_STATE).astype(jnp.float32)),
                 lpad(Cm.reshape(bsz, L, SSD_GROUPS, SSD_STATE).astype(jnp.float32)))
    y = y[:, pad_len:] + d_skip.astype(jnp.float32)[:, None] * xs_h.astype(jnp.float32)
    y = y.reshape(bsz, L, D_SSD) * jax.nn.silu(z.astype(jnp.float32))
    y_ssd = rmsnorm(y.reshape(bsz, L, SSD_GROUPS, D_SSD // SSD_GROUPS),
                    ssd_norm_w.reshape(SSD_GROUPS, D_SSD // SSD_GROUPS))
    y_ssd = y_ssd.reshape(bsz, L, D_SSD).astype(n.dtype)

    y_sc = sc_b * causal_dwconv(sc_c * sc_x, conv_sc_w)

    return jnp.concatenate([y_ssd, y_sc], axis=-1) @ w_out.astype(n.dtype)


def peer_ffn(n, w_q, sub_keys, expert_u, expert_v):
    bsz, L, d = n.shape
    t = n.reshape(-1, d)
    T = t.shape[0]
    q = (t @ w_q.astype(n.dtype)).reshape(T, PEER_HEADS, 2, PEER_HALF)
    s = jnp.einsum('thsd,hskd->thsk', q, sub_keys.astype(n.dtype)).astype(jnp.float32)
    v_half, i_half = lax.top_k(s, PEER_TOPK)
    cand = (v_half[:, :, 0, :, None] + v_half[:, :, 1, None, :]).reshape(T, PEER_HEADS, PEER_TOPK * PEER_TOPK)
    best, pos = lax.top_k(cand, PEER_TOPK)
    i1 = jnp.take_along_axis(i_half[:, :, 0], pos // PEER_TOPK, axis=-1)
    i2 = jnp.take_along_axis(i_half[:, :, 1], pos % PEER_TOPK, axis=-1)
    experts = (i1 * PEER_KEYS + i2).reshape(T, PEER_HEADS * PEER_TOPK)
    gates = jax.nn.softmax(best, axis=-1).reshape(T, PEER_HEADS * PEER_TOPK).astype(n.dtype)

    nb = -(-T // PEER_BLOCK)
    padT = nb * PEER_BLOCK - T
    tp = jnp.pad(t, ((0, padT), (0, 0))).reshape(nb, PEER_BLOCK, d)
    ep = jnp.pad(experts, ((0, padT), (0, 0))).reshape(nb, PEER_BLOCK, -1)
    gp = jnp.pad(gates, ((0, padT), (0, 0))).reshape(nb, PEER_BLOCK, -1)

    def block(args):
        xb, eb, gb = args
        u = expert_u[eb].astype(xb.dtype)
        act = jax.nn.gelu(jnp.einsum('td,tkd->tk', xb, u), approximate=False) * gb
        return jnp.einsum('tk,tkd->td', act, expert_v[eb].astype(xb.dtype))

    out = lax.map(block, (tp, ep, gp))
    return out.reshape(-1, d)[:T].reshape(bsz, L, d)


def setup_inputs(seed: int = 0) -> dict:
    key = jax.random.key(seed)
    ks = jax.random.split(key, 20)

    def nrm(k, shape, scale):
        return jax.random.normal(k, shape, jnp.float32) * scale

    x = nrm(ks[0], (BATCH, SEQ, D_MODEL), 1.0)
    meta_tokens = nrm(ks[1], (N_META, D_MODEL), 1.0)
    g_mix = 1.0 + nrm(ks[2], (DEPTH, D_MODEL), 0.05)
    w_in = nrm(ks[3], (DEPTH, D_MODEL, D_IN), D_MODEL ** -0.5)
    conv_ssd_w = nrm(ks[4], (DEPTH, SSD_CONV, D_XBC), SSD_CONV ** -0.5)
    conv_ssd_b = nrm(ks[5], (DEPTH, D_XBC), 0.02)
    dt0 = jnp.exp(jax.random.uniform(ks[6], (DEPTH, SSD_HEADS), jnp.float32,
                                     minval=math.log(1e-3), maxval=math.log(1e-1)))
    dt_bias = dt0 + jnp.log(-jnp.expm1(-dt0))
    a_log = jnp.log(jax.random.uniform(ks[7], (DEPTH, SSD_HEADS), jnp.float32, minval=1.0, maxval=16.0))
    d_skip = 1.0 + nrm(ks[8], (DEPTH, SSD_HEADS), 0.05)
    ssd_norm_w = 1.0 + nrm(ks[9], (DEPTH, D_SSD), 0.05)
    conv_sc_w = nrm(ks[10], (DEPTH, SC_CONV, D_SC), SC_CONV ** -0.5)
    w_out = nrm(ks[11], (DEPTH, D_MIX, D_MODEL), D_MIX ** -0.5)
    g_ffn = 1.0 + nrm(ks[12], (DEPTH, D_MODEL), 0.05)
    w_q = nrm(ks[13], (DEPTH, D_MODEL, PEER_HEADS * PEER_QDIM), D_MODEL ** -0.5)
    sub_keys = nrm(ks[14], (DEPTH, PEER_HEADS, 2, PEER_KEYS, PEER_HALF), PEER_HALF ** -0.5)
    expert_u = nrm(ks[15], (DEPTH, PEER_EXPERTS, D_MODEL), D_MODEL ** -0.5)
    expert_v = nrm(ks[16], (DEPTH, PEER_EXPERTS, D_MODEL), PEER_HEADS ** -0.5)
    g_final = 1.0 + nrm(ks[17], (D_MODEL,), 0.05)
    return {"x": x, "meta_tokens": meta_tokens, "g_mix": g_mix, "w_in": w_in,
            "conv_ssd_w": conv_ssd_w, "conv_ssd_b": conv_ssd_b, "dt_bias": dt_bias,
            "a_log": a_log, "d_skip": d_skip, "ssd_norm_w": ssd_norm_w,
            "conv_sc_w": conv_sc_w, "w_out": w_out, "g_ffn": g_ffn, "w_q": w_q,
            "sub_keys": sub_keys, "expert_u": expert_u, "expert_v": expert_v,
            "g_final": g_final}


def reference(x, meta_tokens, g_mix, w_in, conv_ssd_w, conv_ssd_b, dt_bias, a_log, d_skip,
              ssd_norm_w, conv_sc_w, w_out, g_ffn, w_q, sub_keys, expert_u, expert_v, g_final):
    bsz = x.shape[0]
    meta = jnp.broadcast_to(meta_tokens[None].astype(x.dtype), (bsz, N_META, D_MODEL))
    h = jnp.concatenate([meta, x], axis=1)
    for l in range(DEPTH):
        h = h + hybrid_mixer(rmsnorm(h, g_mix[l]), w_in[l], conv_ssd_w[l], conv_ssd_b[l],
                             dt_bias[l], a_log[l], d_skip[l], ssd_norm_w[l], conv_sc_w[l], w_out[l])
        h = h + peer_ffn(rmsnorm(h, g_ffn[l]), w_q[l], sub_keys[l], expert_u[l], expert_v[l])
    h = rmsnorm(h, g_final)
    return h[:, N_META:]
```

```python
import numpy as np
from contextlib import ExitStack
import concourse.bass as bass
import concourse.mybir as mybir
from concourse.bass_utils import run_bass_kernel_spmd

F32 = mybir.dt.float32
BF16 = mybir.dt.bfloat16
I32 = mybir.dt.int32
U32 = mybir.dt.uint32
AF = mybir.ActivationFunctionType
ALU = mybir.AluOpType
AX = mybir.AxisListType

D = 1024
SEQ = 2048
NSEQ = 2
NT = SEQ // 128
NMETA = 16
DIN = 5656
NH = 24
HP = 64
NG = 4
DSSD = 1536
EPS = 1e-6
C_Z, C_XBC, C_DT, C_SCB, C_SCC, C_SCX = 0, 1536, 4096, 4120, 4632, 5144
NSLOT = 128
NEXP = 16384
NGB = 10
NEG = -30000.0
STRICT_SAME_ENGINE = True


class Buf:
    __slots__ = ("name", "w", "r", "dsem", "dcnt")

    def __init__(self, name):
        self.name = name
        self.w = None
        self.r = []
        self.dsem = None
        self.dcnt = 0


class Tl:
    def __init__(self, t, name):
        self.t = t
        self.b = Buf(name)

    def __getitem__(self, k):
        return self.t[k]


def _b(x):
    return x.b if isinstance(x, Tl) else x


class Sched:
    def __init__(self, nc, es):
        self.nc = nc
        self.es = es
        self.eng = {"pe": nc.tensor, "act": nc.scalar, "dve": nc.vector, "pool": nc.gpsimd, "sp": nc.sync}
        self.sem = {k: es.enter_context(nc.semaphore("s_" + k)) for k in self.eng}
        self.cnt = {k: 0 for k in self.eng}
        self.seen = {k: {} for k in self.eng}
        self.dbufs = []
        self.nsem = 0

    def _wait(self, e, deps):
        best = {}
        for (s, v) in deps:
            key = id(s)
            if key not in best or v > best[key][1]:
                best[key] = (s, v)
        for key, (s, v) in best.items():
            if self.seen[e].get(key, 0) >= v:
                continue
            self.eng[e].wait_ge(s, v)
            self.seen[e][key] = v

    def _deps(self, r, w, c, own=None):
        deps = []
        for b in r:
            if b.w is not None:
                deps.append(b.w)
        for b in c:
            if b.w is not None:
                deps.append(b.w)
        for b in w:
            if b.w is not None and (STRICT_SAME_ENGINE or b.w[0] is not own):
                deps.append(b.w)
            deps.extend(x for x in b.r if STRICT_SAME_ENGINE or x[0] is not own)
        return deps

    def op(self, e, fn, r=(), w=(), c=()):
        r = [_b(x) for x in r]
        w = [_b(x) for x in w]
        c = [_b(x) for x in c]
        deps = self._deps(r, w, c, own=self.sem[e])
        if e == "pe":
            deps = [d for d in deps if d[0] is not self.sem["pe"]]
        self._wait(e, deps)
        inst = fn(self.eng[e])
        self.cnt[e] += 1
        inst.then_inc(self.sem[e], 1)
        t = (self.sem[e], self.cnt[e])
        for b in w:
            b.w = t
            b.r = []
        for b in r:
            b.r.append(t)
        return inst

    def dma(self, q, fn, dst, r=(), c=()):
        dst = _b(dst)
        r = [_b(x) for x in r]
        c = [_b(x) for x in c]
        deps = self._deps(r, [dst], c)
        self._wait(q, deps)
        if dst.dsem is None:
            dst.dsem = self.es.enter_context(self.nc.semaphore("d%d_%s" % (self.nsem, dst.name)))
            self.nsem += 1
            self.dbufs.append(dst)
        inst = fn(self.eng[q])
        dst.dcnt += 16
        inst.then_inc(dst.dsem, 16)
        t = (dst.dsem, dst.dcnt)
        dst.w = t
        dst.r = []
        for b in r:
            b.r.append(t)
        return inst

    def barrier(self):
        allv = [(self.sem[k], self.cnt[k]) for k in self.eng if self.cnt[k] > 0]
        allv += [(b.dsem, b.dcnt) for b in self.dbufs]
        for e in self.eng:
            self._wait(e, [d for d in allv if d[0] is not self.sem[e]])

    def final_wait(self, e, bufs):
        self._wait(e, [(_b(b).dsem, _b(b).dcnt) for b in bufs])


class Pool:
    def __init__(self, tiles):
        self.tiles = tiles
        self.i = 0

    def get(self):
        t = self.tiles[self.i % len(self.tiles)]
        self.i += 1
        return t


def build(debug=None, nseq=NSEQ, ntile=NT):
    nc = bass.Bass("TRN2", target_bir_lowering=False)
    dbgA = debug == "A"
    x_d = nc.dram_tensor("x", [NSEQ, SEQ, D], F32, kind="ExternalInput")
    meta_d = nc.dram_tensor("meta", [NMETA, D], F32, kind="ExternalInput")
    win_d = nc.dram_tensor("w_in", [D, DIN], F32, kind="ExternalInput")
    wout_d = nc.dram_tensor("w_out", [2048, D], F32, kind="ExternalInput")
    wq_d = nc.dram_tensor("w_q", [D, 2048], F32, kind="ExternalInput")
    skt_d = nc.dram_tensor("skt", [128, 2048], F32, kind="ExternalInput")
    eu_d = nc.dram_tensor("expert_u", [NEXP, D], F32, kind="ExternalInput")
    ev_d = nc.dram_tensor("expert_v", [NEXP, D], F32, kind="ExternalInput")
    pp_d = nc.dram_tensor("pp", [128, 136], F32, kind="ExternalInput")
    bpa_d = nc.dram_tensor("bpa", [1, 72], F32, kind="ExternalInput")
    bpb_d = nc.dram_tensor("bpb", [1, 2048], F32, kind="ExternalInput")
    cst_d = nc.dram_tensor("cst", [128, 528], F32, kind="ExternalInput")
    out_d = nc.dram_tensor("out", [NSEQ, SEQ, D], F32, kind="ExternalOutput")
    yt_d = nc.dram_tensor("yt_scr", [NSEQ * NT, 128, 2048], BF16,
                          **({"kind": "ExternalOutput"} if dbgA else {}))

    uv_d = nc.dram_tensor("uv_scr", [NEXP, 2 * D], BF16)
    dbg_d = {}
    if dbgA:
        for nm, shp, dt_ in [("dts", [128, 192], F32), ("xsB", [128, 2048], BF16), ("xdt", [128, 1536], BF16),
                             ("cbT", [128, 512], F32), ("t1", [128, 1536], F32), ("yb", [128, 1536], F32),
                             ("St", [128, 1536], F32), ("Smeta", [128, 1536], F32), ("sz", [128, 1536], BF16),
                             ("Dm1", [128, 384], F32), ("MT1", [128, 384], BF16), ("xact", [128, 2560], BF16)]:
            dbg_d[nm] = nc.dram_tensor("dbg_" + nm, shp, dt_, kind="ExternalOutput")
    dbg_bufs = []
    with ExitStack() as es:
        S = Sched(nc, es)

        def sb(stack, name, shape, dt):
            return Tl(stack.enter_context(nc.sbuf_tensor("sb_" + name, shape, dt)), name)

        ps = [Tl(es.enter_context(nc.psum_tensor("ps%d" % i, [128, 512], F32)), "ps%d" % i) for i in range(8)]
        yt_b = Buf("yt_scr")
        out_b = Buf("out")

        cst = sb(es, "cst", [128, 528], F32)
        identb = sb(es, "identb", [128, 128], BF16)
        S.dma("sp", lambda e: e.dma_start(out=cst[:, :], in_=cst_d.ap()), dst=cst)
        S.op("dve", lambda e: e.tensor_copy(out=identb[:, :], in_=cst[:, 0:128]), r=[cst], w=[identb])
        ident = cst
        TRIU0, ONES0, NEGM0, IOTA0 = 128, 256, 384, 512
        uv_bufs = [Buf("uv%d" % i) for i in range(4)]
        UVCH = 1024
        uv_jobs = [(r0, half) for r0 in range(0, NEXP, UVCH) for half in range(2)]
        uv_state = {"i": 0}

        def uv_issue(n):
            for _ in range(n):
                i = uv_state["i"]
                if dbgA or i >= len(uv_jobs):
                    return
                r0, half = uv_jobs[i]
                src_d = eu_d if half == 0 else ev_d
                S.dma("pool", lambda e: e.dma_start(out=uv_d[r0:r0 + UVCH, half * D:(half + 1) * D], in_=src_d[r0:r0 + UVCH, :]),
                      dst=uv_bufs[i % 4])
                uv_state["i"] = i + 1

        with ExitStack() as ea:
            fpool = Pool(ps[0:2])
            bpool = Pool(ps[2:5])
            win = sb(ea, "win", [128, 8, DIN], BF16)
            pp = sb(ea, "pp", [128, 136], F32)
            bpa = sb(ea, "bpa", [128, 72], F32)
            A_bc = sb(ea, "A_bc", [128, NH], F32)
            S.dma("sp", lambda e: e.dma_start(out=pp[:, :], in_=pp_d.ap()), dst=pp)
            S.dma("sp", lambda e: e.dma_start(out=bpa[:, :], in_=bpa_d.ap().partition_broadcast(128)), dst=bpa)
            GM0, CW0, CB0, SCW0, NW0 = 0, 8, 88, 108, 120
            DTB0, AL0, DSK0 = 0, 24, 48
            S.op("act", lambda e: e.activation(out=A_bc[:, :], in_=bpa[:, AL0:AL0 + NH], func=AF.Exp), r=[bpa], w=[A_bc])
            S.op("dve", lambda e: e.tensor_scalar(out=A_bc[:, :], in0=A_bc[:, :], scalar1=-1.0, scalar2=None, op0=ALU.mult),
                 r=[A_bc], w=[A_bc])
            with ExitStack() as est:
                stg = [sb(est, "stg%d" % i, [128, 2828], F32) for i in range(2)]
                n = 0
                for k in range(8):
                    for hb in range(2):
                        st = stg[n % 2]
                        S.dma("sp", lambda e, st=st, k=k, hb=hb: e.dma_start(
                            out=st[:, :], in_=win_d[k * 128:(k + 1) * 128, hb * 2828:(hb + 1) * 2828]), dst=st)
                        if n % 2 == 0:
                            S.op("act", lambda e, st=st, k=k, hb=hb: e.activation(
                                out=win[:, k, hb * 2828:(hb + 1) * 2828], in_=st[:, :], func=AF.Copy,
                                scale=pp[:, GM0 + k:GM0 + k + 1]), r=[st], w=[win], c=[pp])
                        else:
                            S.op("dve", lambda e, st=st, k=k, hb=hb: e.tensor_scalar(
                                out=win[:, k, hb * 2828:(hb + 1) * 2828], in0=st[:, :],
                                scalar1=pp[:, GM0 + k:GM0 + k + 1], scalar2=None, op0=ALU.mult), r=[st], w=[win], c=[pp])
                        n += 1
                S.barrier()
            xts = [sb(ea, "xt%d" % i, [128, D], F32) for i in range(1)]
            xn = sb(ea, "xn", [128, D], BF16)
            junk2 = sb(ea, "junk2", [128, 384], BF16)
            sm = sb(ea, "sm", [128, 4], F32)
            smk = sb(ea, "smk", [128, 16], F32)
            nT = sb(ea, "nT", [128, 8, 128], BF16)
            xpres = [sb(ea, "xpre%d" % i, [128, 20, 131], BF16) for i in range(2)]
            hxs = [sb(ea, "hx%d" % i, [128, 20, 3], BF16) for i in range(3)]
            hcs = [sb(ea, "hc%d" % i, [128, 4, 2], F32) for i in range(3)]
            xacts = [sb(ea, "xact%d" % i, [128, 20, 128], BF16) for i in range(2)]
            cacc = [sb(ea, "cacc%d" % i, [128, 128], F32) for i in range(8)]
            scbs = [sb(ea, "scb%d" % i, [128, 4, 128], BF16) for i in range(2)]
            scc = sb(ea, "scc", [128, 4, 128], F32)
            cxs = [sb(ea, "cx%d" % i, [128, 4, 130], F32) for i in range(2)]
            sacc = sb(ea, "sacc", [128, 4, 128], F32)
            szs = [sb(ea, "sz%d" % i, [128, DSSD], BF16) for i in range(3)]
            xsB = sb(ea, "xsB", [128, 2048], BF16)
            xdt = sb(ea, "xdt", [128, DSSD], BF16)
            xdd = sb(ea, "xdd", [128, DSSD], BF16)
            dtfs = [sb(ea, "dtf%d" % i, [128, 3, NH], F32) for i in range(3)]
            dts = sb(ea, "dts", [128, 8, NH], F32)
            rhsA = [sb(ea, "rhsA%d" % i, [128, 384], F32) for i in range(2)]
            Dm = [sb(ea, "Dm%d" % i, [128, 384], F32) for i in range(2)]
            MT = [sb(ea, "MT%d" % i, [128, 384], BF16) for i in range(3)]
            cbT = sb(ea, "cbT", [128, 512], F32)
            t1 = sb(ea, "t1", [128, DSSD], F32)
            yn = sb(ea, "yn", [128, DSSD], BF16)
            YTs = [sb(ea, "YT%d" % i, [128, 16, 128], BF16) for i in range(2)]
            St = sb(ea, "St", [128, DSSD], F32)
            Stmp = sb(ea, "Stmp", [128, DSSD], F32)
            t1b = sb(ea, "t1b", [128, DSSD], BF16)
            yb = t1
            Sbf = sb(ea, "Sbf", [128, DSSD], BF16)
            Smeta = sb(ea, "Smeta", [128, DSSD], F32)
            halo_x = sb(ea, "halo_x", [128, 20, 3], F32)
            halo_c = sb(ea, "halo_c", [128, 4, 2], F32)
            state = {"n": 0}

            def frontA(seq, ti, T, meta, par, sj=0, jg=0):
                first = (ti == 0) and not meta
                last = (ti == ntile - 1) and not meta
                xpre = xpres[jg % 2]
                cx = cxs[jg % 2]
                scb = scbs[jg % 2]
                junk = xn
                xt = xts[0]
                YT = YTs[par]
                xact = xacts[par]
                sz = szs[sj]
                dtf = dtfs[sj]
                pool = fpool
                src = meta_d.ap() if meta else x_d[seq, ti * 128:(ti + 1) * 128, :]
                S.dma("sp", lambda e: e.dma_start(out=xt[0:T, :], in_=src), dst=xt)
                S.op("act", lambda e: e.activation(out=junk[0:T, 0:D], in_=xt[0:T, :], func=AF.Square,
                                                   accum_out=sm[0:T, 0:1]), r=[xt], w=[xn, sm])
                S.op("act", lambda e: e.activation(out=sm[0:T, 1:2], in_=sm[0:T, 0:1], func=AF.Ln, scale=1.0 / D, bias=EPS),
                     r=[sm], w=[sm])
                S.op("act", lambda e: e.activation(out=sm[0:T, 2:3], in_=sm[0:T, 1:2], func=AF.Exp, scale=-0.5),
                     r=[sm], w=[sm])
                S.op("act", lambda e: e.activation(out=xn[0:T, :], in_=xt[0:T, :], func=AF.Copy, scale=sm[0:T, 2:3]),
                     r=[xt, sm], w=[xn])
                yield
                pt = pool.get()
                ptb = pt.t[:].bitcast(BF16)
                for k in range(8):
                    S.op("pe", lambda e, k=k: e.transpose(out=ptb[:, k * 128:k * 128 + T], in_=xn[0:T, k * 128:(k + 1) * 128],
                                                          identity=identb[0:T, 0:T]), r=[xn], w=[pt], c=[identb])
                yield
                S.op("dve", lambda e: e.tensor_copy(out=nT[:, :, 0:T], in_=ptb.rearrange("p (k t) -> p k t", k=8)[:, :, 0:T]),
                     r=[pt], w=[nT])
                yield

                fmres = {}

                def fm_bank(col0, nchunk=4):
                    p = pool.get()
                    fmres["p"] = p
                    for j in range(nchunk):
                        for k in range(8):
                            S.op("pe", lambda e, j=j, k=k: e.matmul(
                                p[:, j * 128:j * 128 + T], lhsT=win[:, k, col0 + j * 128:col0 + (j + 1) * 128],
                                rhs=nT[:, k, 0:T], start=(k == 0), stop=(k == 7)), r=[nT], w=[p], c=[win])
                        yield

                def p3(p):
                    return p.t[:].rearrange("p (j t) -> p j t", j=4)[:, :, 0:T]

                for bq in range(5):
                    yield from fm_bank(C_XBC + bq * 512)
                    p = fmres["p"]
                    eng = "act" if bq % 2 == 0 else "dve"
                    if eng == "act":
                        S.op("act", lambda e, p=p, bq=bq: e.activation(out=xpre[:, 4 * bq:4 * bq + 4, 3:3 + T], in_=p3(p), func=AF.Copy),
                             r=[p], w=[xpre])
                    else:
                        S.op("dve", lambda e, p=p, bq=bq: e.tensor_copy(out=xpre[:, 4 * bq:4 * bq + 4, 3:3 + T], in_=p3(p)),
                             r=[p], w=[xpre])
                    yield
                if not meta:
                    yield from fm_bank(C_SCB)
                    p = fmres["p"]
                    S.op("act", lambda e, p=p: e.activation(out=scb[:, :, 0:T], in_=p3(p), func=AF.Copy), r=[p], w=[scb])
                yield from fm_bank(C_SCC)
                p = fmres["p"]
                S.op("act", lambda e, p=p: e.activation(out=scc[:, :, 0:T], in_=p3(p), func=AF.Copy), r=[p], w=[scc])
                yield from fm_bank(C_SCX)
                p = fmres["p"]
                S.op("dve", lambda e, p=p: e.tensor_tensor(out=cx[:, :, 2:2 + T], in0=scc[:, :, 0:T], in1=p3(p), op=ALU.mult),
                     r=[p, scc], w=[cx])
                yield
                p = pool.get()
                for k in range(8):
                    S.op("pe", lambda e, k=k, p=p: e.matmul(p[0:T, 0:NH], lhsT=nT[:, k, 0:T], rhs=win[:, k, C_DT:C_DT + NH],
                                                            start=(k == 0), stop=(k == 7)), r=[nT], w=[p], c=[win])
                S.op("dve", lambda e, p=p: e.tensor_tensor(out=dtf[0:T, 2, :], in0=p[0:T, 0:NH], in1=bpa[0:T, DTB0:DTB0 + NH], op=ALU.add),
                     r=[p], w=[dtf], c=[bpa])
                S.op("act", lambda e: e.activation(out=dtf[0:T, 2, :], in_=dtf[0:T, 2, :], func=AF.Exp), r=[dtf], w=[dtf])
                S.op("act", lambda e: e.activation(out=dtf[0:T, 0, :], in_=dtf[0:T, 2, :], func=AF.Ln, bias=1.0), r=[dtf], w=[dtf])
                S.op("dve", lambda e: e.tensor_tensor(out=dtf[0:T, 1, :], in0=dtf[0:T, 0, :], in1=A_bc[0:T, :], op=ALU.mult),
                     r=[dtf], w=[dtf], c=[A_bc])
                yield
                if not meta:
                    for zb in range(3):
                        p = pool.get()
                        for k in range(8):
                            S.op("pe", lambda e, k=k, p=p, zb=zb: e.matmul(
                                p[0:T, :], lhsT=nT[:, k, 0:T], rhs=win[:, k, C_Z + zb * 512:C_Z + (zb + 1) * 512],
                                start=(k == 0), stop=(k == 7)), r=[nT], w=[p], c=[win])
                            if k % 4 == 3:
                                yield
                        S.op("act", lambda e, p=p, zb=zb: e.activation(out=sz[0:T, zb * 512:(zb + 1) * 512], in_=p[0:T, :], func=AF.Silu),
                             r=[p], w=[sz])
                        yield
                if not meta and not last:
                    S.op("act", lambda e: e.activation(out=hxs[sj][:, :, :], in_=xpre[:, :, T:T + 3], func=AF.Copy), r=[xpre], w=[hxs[sj]])
                    S.op("act", lambda e: e.activation(out=hcs[sj][:, :, :], in_=cx[:, :, T:T + 2], func=AF.Copy), r=[cx], w=[hcs[sj]])
                yield "mid"
                if meta:
                    S.op("pool", lambda e: e.memset(xpre[:, :, 0:3], 0.0), w=[xpre])
                    S.op("pool", lambda e: e.memset(cx[:, :, 0:2], 0.0), w=[cx])
                elif first:
                    S.op("act", lambda e: e.activation(out=xpre[:, :, 0:3], in_=halo_x[:, :, :], func=AF.Copy), r=[halo_x], w=[xpre])
                    S.op("act", lambda e: e.activation(out=cx[:, :, 0:2], in_=halo_c[:, :, :], func=AF.Copy), r=[halo_c], w=[cx])
                else:
                    hp = (sj + 2) % 3
                    S.op("act", lambda e: e.activation(out=xpre[:, :, 0:3], in_=hxs[hp][:, :, :], func=AF.Copy), r=[hxs[hp]], w=[xpre])
                    S.op("act", lambda e: e.activation(out=cx[:, :, 0:2], in_=hcs[hp][:, :, :], func=AF.Copy), r=[hcs[hp]], w=[cx])
                def conv_head(g):
                    for j in range(4):
                        k = 4 * g + j
                        ca = cacc[k % 8]
                        S.op("act", lambda e, k=k, ca=ca: e.activation(
                            out=ca[:, 0:T], in_=xpre[:, k, 3:3 + T], func=AF.Identity,
                            scale=pp[:, CW0 + 4 * k + 3:CW0 + 4 * k + 4], bias=pp[:, CB0 + k:CB0 + k + 1]), r=[xpre], w=[ca], c=[pp])

                def conv_taps(g):
                    for w_ in range(3):
                        for j in range(4):
                            k = 4 * g + j
                            ca = cacc[k % 8]
                            S.op("dve", lambda e, k=k, ca=ca, w_=w_: e.scalar_tensor_tensor(
                                out=ca[:, 0:T], in0=xpre[:, k, w_:w_ + T], scalar=pp[:, CW0 + 4 * k + w_:CW0 + 4 * k + w_ + 1],
                                in1=ca[:, 0:T], op0=ALU.mult, op1=ALU.add), r=[xpre, ca], w=[ca], c=[pp])

                def conv_tail(g):
                    for j in range(4):
                        k = 4 * g + j
                        ca = cacc[k % 8]
                        S.op("act", lambda e, k=k, ca=ca: e.activation(out=xact[:, k, 0:T], in_=ca[:, 0:T], func=AF.Silu), r=[ca], w=[xact])

                conv_head(0)
                yield
                for g in range(5):
                    if g + 1 < 5:
                        conv_head(g + 1)
                    conv_taps(g)
                    yield
                    conv_tail(g)
                    yield
                for k in range(4):
                    S.op("act", lambda e, k=k: e.activation(
                        out=sacc[:, k, 0:T], in_=cx[:, k, 2:2 + T], func=AF.Copy, scale=pp[:, SCW0 + 3 * k + 2:SCW0 + 3 * k + 3]),
                        r=[cx], w=[sacc], c=[pp])
                    for w_ in range(2):
                        S.op("dve", lambda e, k=k, w_=w_: e.scalar_tensor_tensor(
                            out=sacc[:, k, 0:T], in0=cx[:, k, w_:w_ + T], scalar=pp[:, SCW0 + 3 * k + w_:SCW0 + 3 * k + w_ + 1],
                            in1=sacc[:, k, 0:T], op0=ALU.mult, op1=ALU.add), r=[cx, sacc], w=[sacc], c=[pp])
                if not meta:
                    S.op("pool", lambda e: e.tensor_tensor(out=YT[:, 12:16, 0:T], in0=sacc[:, :, 0:T], in1=scb[:, :, 0:T], op=ALU.mult),
                         r=[sacc, scb], w=[YT])
                yield
                if meta:
                    S.op("pool", lambda e: e.tensor_copy(out=halo_x[:, :, :], in_=xpre[:, :, T:T + 3]), r=[xpre], w=[halo_x])
                    S.op("pool", lambda e: e.tensor_copy(out=halo_c[:, :, :], in_=cx[:, :, T:T + 2]), r=[cx], w=[halo_c])
            def backA(seq, ti, T, meta, par, pump, sj=0):
                first = (ti == 0) and not meta
                last = (ti == ntile - 1) and not meta
                YT = YTs[par]
                xact = xacts[par]
                sz = szs[sj]
                dtf = dtfs[sj]
                pool = bpool
                for hb in range(2):
                    pt = pool.get()
                    ptb = pt.t[:].bitcast(BF16)
                    for j in range(8):
                        cc = hb * 8 + j
                        S.op("pe", lambda e, j=j, cc=cc, ptb=ptb, pt=pt: e.transpose(
                            out=ptb[0:T, j * 128:(j + 1) * 128], in_=xact[:, cc, 0:T], identity=identb[:, :]),
                            r=[xact], w=[pt], c=[identb])
                    if hb == 0:
                        S.op("act", lambda e, ptb=ptb, pt=pt: e.activation(out=xsB[0:T, 0:1024], in_=ptb[0:T, :], func=AF.Copy), r=[pt], w=[xsB])
                    else:
                        S.op("dve", lambda e, ptb=ptb, pt=pt: e.tensor_copy(out=xsB[0:T, 1024:2048], in_=ptb[0:T, :]), r=[pt], w=[xsB])
                pump(2)
                p = pool.get()
                S.op("pe", lambda e, p=p: e.matmul(p[0:T, 0:NH], lhsT=cst[0:T, TRIU0:TRIU0 + T], rhs=dtf[0:T, 1, :], start=True, stop=True),
                     r=[dtf], w=[p], c=[cst])
                S.op("pe", lambda e, p=p: e.matmul(p[0:T, 32:32 + NH], lhsT=cst[0:T, ONES0:ONES0 + T], rhs=dtf[0:T, 1, :], start=True, stop=True),
                     r=[dtf], w=[p], c=[cst])
                S.op("dve", lambda e, p=p: e.tensor_copy(out=dts[0:T, 2, :], in_=p[0:T, 0:NH]), r=[p], w=[dts])
                S.op("act", lambda e, p=p: e.activation(out=dts[0:T, 3, :], in_=p[0:T, 0:NH], func=AF.Exp), r=[p], w=[dts])
                S.op("act", lambda e, p=p: e.activation(out=dts[0:T, 5, :], in_=p[0:T, 32:32 + NH], func=AF.Exp), r=[p], w=[dts])
                S.op("dve", lambda e, p=p: e.tensor_tensor(out=dts[0:T, 6, :], in0=p[0:T, 32:32 + NH], in1=dts[0:T, 2, :], op=ALU.subtract),
                     r=[p, dts], w=[dts])
                S.op("act", lambda e: e.activation(out=dts[0:T, 4, :], in_=dts[0:T, 6, :], func=AF.Exp), r=[dts], w=[dts])

                pump(2)

                def hb3(ap2, nh=NH):
                    return ap2.rearrange("p (h q) -> p h q", h=nh)

                def bch(ap2, nh=NH):
                    return ap2.unsqueeze(2).to_broadcast([T, nh, HP])

                S.op("dve", lambda e: e.tensor_tensor(out=hb3(xdt[0:T, :]), in0=hb3(xsB[0:T, 0:DSSD]), in1=bch(dtf[0:T, 0, :]), op=ALU.mult),
                     r=[xsB, dtf], w=[xdt])
                S.op("pool", lambda e: e.tensor_tensor(out=hb3(xdd[0:T, :]), in0=hb3(xdt[0:T, :]), in1=bch(dts[0:T, 4, :]), op=ALU.mult),
                     r=[xdt, dts], w=[xdd])
                if not meta:
                    for g in range(NG):
                        p = pool.get()
                        S.op("pe", lambda e, p=p, g=g: e.matmul(p[0:T, 0:384], lhsT=xact[:, 16 + g, 0:T], rhs=Sbf[:, g * 384:(g + 1) * 384],
                                                                start=True, stop=True), r=[xact, Sbf], w=[p])
                        S.op("dve", lambda e, p=p, g=g: e.tensor_tensor(
                            out=hb3(t1[0:T, g * 384:(g + 1) * 384], 6), in0=hb3(p[0:T, 0:384], 6),
                            in1=bch(dts[0:T, 3, 6 * g:6 * g + 6], 6), op=ALU.mult), r=[p, dts], w=[t1])
                        pump(2)
                    S.op("pool", lambda e: e.tensor_tensor(out=hb3(t1b[0:T, :]), in0=hb3(xsB[0:T, 0:DSSD]),
                                                           in1=bch(bpa[0:T, DSK0:DSK0 + NH]), op=ALU.mult), r=[xsB], w=[t1b], c=[bpa])
                    S.op("dve", lambda e: e.tensor_tensor(out=t1[0:T, :], in0=t1[0:T, :], in1=t1b[0:T, :], op=ALU.add), r=[t1, t1b], w=[t1])
                if not last:
                    dstS = Smeta if meta else St
                    if not meta:
                        S.op("pool", lambda e: e.tensor_tensor(out=hb3(Stmp[:, :]), in0=hb3(St[:, :]),
                                                               in1=dts[:, 5, :].unsqueeze(2).to_broadcast([128, NH, HP]), op=ALU.mult),
                             r=[St, dts], w=[Stmp])
                    for g in range(NG):
                        p = pool.get()
                        S.op("pe", lambda e, p=p, g=g: e.matmul(p[:, 0:384], lhsT=xsB[0:T, DSSD + g * 128:DSSD + (g + 1) * 128],
                                                                rhs=xdd[0:T, g * 384:(g + 1) * 384], start=True, stop=True),
                             r=[xsB, xdd], w=[p])
                        if meta:
                            S.op("dve", lambda e, g=g, p=p: e.tensor_copy(out=dstS[:, g * 384:(g + 1) * 384], in_=p[:, 0:384]),
                                 r=[p], w=[dstS])
                        else:
                            S.op("dve", lambda e, g=g, p=p: e.tensor_tensor(out=St[:, g * 384:(g + 1) * 384], in0=p[:, 0:384],
                                                                            in1=Stmp[:, g * 384:(g + 1) * 384], op=ALU.add),
                                 r=[p, Stmp], w=[St])
                        pump(2)
                    if not meta:
                        S.op("act", lambda e: e.activation(out=Sbf[:, :], in_=St[:, :], func=AF.Copy), r=[St], w=[Sbf])
                if not meta:
                    p = pool.get()
                    for g in range(NG):
                        S.op("pe", lambda e, p=p, g=g: e.matmul(p[0:T, g * 128:g * 128 + T], lhsT=xact[:, 12 + g, 0:T], rhs=xact[:, 16 + g, 0:T],
                                                                start=True, stop=True), r=[xact], w=[p])
                    S.op("act", lambda e, p=p: e.activation(out=cbT[0:T, :], in_=p[0:T, :], func=AF.Copy), r=[p], w=[cbT])
                    pump(2)
                    ybanks = ps[5:8]
                    dbank = {}

                    def unit_head(u):
                        ra = rhsA[u % 2]
                        for j in range(3):
                            h = 3 * u + j
                            S.op("act", lambda e, j=j, h=h: e.activation(
                                out=ra[0:T, j * T:(j + 1) * T], in_=cst[0:T, TRIU0:TRIU0 + T], func=AF.Copy,
                                scale=dtf[0:T, 1, h:h + 1]), r=[dtf], w=[ra], c=[cst])
                        p = pool.get()
                        dbank[u] = p
                        S.op("pe", lambda e: e.matmul(p[0:T, 0:3 * T], lhsT=cst[0:T, ONES0:ONES0 + T], rhs=ra[0:T, 0:3 * T],
                                                      start=True, stop=True), r=[ra], w=[p], c=[cst])

                    unit_head(0)
                    pump(2)
                    for u in range(8):
                        g = u // 2
                        dm = Dm[u % 2]
                        mt = MT[u % 3]
                        if u + 1 < 8:
                            unit_head(u + 1)
                        p = dbank[u]
                        for j in range(3):
                            h = 3 * u + j
                            S.op("dve", lambda e, p=p, dm=dm, j=j, h=h: e.scalar_tensor_tensor(
                                out=dm[0:T, j * T:(j + 1) * T], in0=p[0:T, j * T:(j + 1) * T], scalar=dts[0:T, 2, h:h + 1],
                                in1=cst[0:T, NEGM0:NEGM0 + T], op0=ALU.subtract, op1=ALU.add), r=[p, dts], w=[dm], c=[cst])
                        pump(2)
                        S.op("act", lambda e, dm=dm: e.activation(out=dm[0:T, 0:3 * T], in_=dm[0:T, 0:3 * T], func=AF.Exp), r=[dm], w=[dm])
                        pump(2)
                        S.op("dve", lambda e, dm=dm, mt=mt, g=g: e.tensor_tensor(
                            out=mt[0:T, 0:3 * T].rearrange("p (j l) -> p j l", j=3),
                            in0=dm[0:T, 0:3 * T].rearrange("p (j l) -> p j l", j=3),
                            in1=cbT[0:T, g * 128:g * 128 + T].unsqueeze(1).to_broadcast([T, 3, T]), op=ALU.mult),
                            r=[dm, cbT], w=[mt])
                        for j in range(3):
                            h = 3 * u + j
                            yp = ybanks[h // 8]
                            S.op("pe", lambda e, yp=yp, mt=mt, j=j, h=h: e.matmul(
                                yp[0:T, (h % 8) * 64:(h % 8 + 1) * 64], lhsT=mt[0:T, j * T:(j + 1) * T],
                                rhs=xdt[0:T, h * 64:(h + 1) * 64], start=True, stop=True), r=[mt, xdt], w=[yp])
                        pump(2)
                    pump.mid()
                    for bq in range(3):
                        S.op("dve", lambda e, bq=bq: e.tensor_tensor(out=yb[0:T, bq * 512:(bq + 1) * 512], in0=ybanks[bq][0:T, :],
                                                                     in1=t1[0:T, bq * 512:(bq + 1) * 512], op=ALU.add),
                             r=[ybanks[bq], t1], w=[yb])
                        pump(2)
                    S.op("dve", lambda e: e.tensor_tensor(out=yb[0:T, :], in0=yb[0:T, :], in1=sz[0:T, :], op=ALU.mult), r=[yb, sz], w=[yb])
                    for g in range(NG):
                        S.op("act", lambda e, g=g: e.activation(out=junk2[0:T, 0:384], in_=yb[0:T, g * 384:(g + 1) * 384], func=AF.Square,
                                                                accum_out=smk[0:T, 4 + g:5 + g]), r=[yb], w=[smk, junk2])
                    S.op("act", lambda e: e.activation(out=smk[0:T, 8:12], in_=smk[0:T, 4:8], func=AF.Ln, scale=1.0 / 384, bias=EPS), r=[smk], w=[smk])
                    S.op("act", lambda e: e.activation(out=smk[0:T, 12:16], in_=smk[0:T, 8:12], func=AF.Exp, scale=-0.5), r=[smk], w=[smk])
                    S.op("dve", lambda e: e.tensor_tensor(
                        out=yn[0:T, :].rearrange("p (g q) -> p g q", g=NG), in0=yb[0:T, :].rearrange("p (g q) -> p g q", g=NG),
                        in1=smk[0:T, 12:16].unsqueeze(2).to_broadcast([T, NG, 384]), op=ALU.mult), r=[yb, smk], w=[yn])
                    pump(2)
                    for hb in range(2):
                        nchk = 8 if hb == 0 else 4
                        pt = pool.get()
                        ptb = pt.t[:].bitcast(BF16)
                        for j in range(nchk):
                            cc = hb * 8 + j
                            S.op("pe", lambda e, j=j, cc=cc, ptb=ptb: e.transpose(
                                out=ptb[:, j * 128:j * 128 + T], in_=yn[0:T, cc * 128:(cc + 1) * 128], identity=identb[0:T, 0:T]),
                                r=[yn], w=[pt], c=[identb])
                        S.op("act", lambda e, ptb=ptb, hb=hb, nchk=nchk: e.activation(
                            out=YT[:, hb * 8:hb * 8 + nchk, 0:T], in_=ptb.rearrange("p (k t) -> p k t", k=8)[:, 0:nchk, 0:T], func=AF.Copy),
                            r=[pt], w=[YT])
                        pump(2)
                    S.dma("pool", lambda e: e.dma_start(out=yt_d[seq * NT + ti, :, :], in_=YT[:, :, :].rearrange("p k t -> p (k t)")),
                          dst=yt_b, r=[YT])
                pump(10 ** 6)

            def dump(nm, tl, ap):
                bb = Buf("dbg_" + nm)
                dbg_bufs.append(bb)
                S.dma("pool", lambda e: e.dma_start(out=dbg_d[nm].ap(), in_=ap), dst=bb, r=[tl])

            class FrontRun:
                def __init__(self, gen):
                    self.g = gen
                    self.done = gen is None
                    self.at_mid = False

                def step(self, n, stop_mid=False):
                    for _ in range(n):
                        if self.done or (stop_mid and self.at_mid):
                            return
                        try:
                            v = next(self.g)
                            if v == "mid":
                                self.at_mid = True
                        except StopIteration:
                            self.done = True

                def flush(self):
                    while not self.done:
                        self.step(1000)

            class Pumper:
                def __init__(self, fa, fb):
                    self.fa, self.fb = fa, fb

                def __call__(self, n=1):
                    if n >= 10 ** 6:
                        if self.fa is not None:
                            self.fa.flush()
                        if self.fb is not None:
                            self.fb.step(10 ** 6, stop_mid=True)
                        return
                    for _ in range(n):
                        if self.fa is not None:
                            self.fa.step(1)
                        if self.fb is not None:
                            self.fb.step(2, stop_mid=True)

                def mid(self):
                    pass

            nopump = Pumper(None, None)
            for _ in frontA(0, 0, NMETA, True, 0):
                pass
            backA(0, 0, NMETA, True, 0, nopump)
            if dbgA:
                dump("Smeta", Smeta, Smeta[:, :])
            tiles = [(sq, ti) for sq in range(nseq) for ti in range(ntile)]
            runs = {}

            def get_run(j):
                if j >= len(tiles):
                    return None
                if j not in runs:
                    runs[j] = FrontRun(frontA(tiles[j][0], tiles[j][1], 128, False, (j + 1) % 2, sj=j % 3, jg=j))
                return runs[j]

            get_run(0).flush()
            if get_run(1) is not None:
                get_run(1).step(10 ** 6, stop_mid=True)
            for i, (sq, ti) in enumerate(tiles):
                par = (i + 1) % 2
                if ti == 0:
                    S.op("pool", lambda e: e.tensor_copy(out=St[:, :], in_=Smeta[:, :]), r=[Smeta], w=[St])
                    S.op("act", lambda e: e.activation(out=Sbf[:, :], in_=Smeta[:, :], func=AF.Copy), r=[Smeta], w=[Sbf])
                uv_issue(1)
                pump = Pumper(get_run(i + 1), get_run(i + 2))
                backA(sq, ti, 128, False, par, pump, sj=i % 3)
                if dbgA and sq == 0 and ti == 0:
                    dump("dts", dts, dts[:, :, :].rearrange("p a h -> p (a h)"))
                    dump("xsB", xsB, xsB[:, :]); dump("xdt", xdt, xdt[:, :]); dump("cbT", cbT, cbT[:, :])
                    dump("t1", t1, t1[:, :]); dump("yb", yb, yb[:, :]); dump("St", St, St[:, :]); dump("sz", szs[i % 3], szs[i % 3][:, :])
                    dump("Dm1", Dm[1], Dm[1][:, :]); dump("MT1", MT[1], MT[1][:, :])
                    dump("xact", xacts[par], xacts[par][:, :, :].rearrange("p k t -> p (k t)"))
            uv_issue(10 ** 6)
            S.barrier()

        if dbgA:
            S.final_wait("sp", [yt_b] + dbg_bufs)
            return nc
        with ExitStack() as eb:
            gpool = Pool(ps[0:4])
            ffnbs = [[ps[4], ps[5]], [ps[6], ps[7]]]
            wout = sb(eb, "wout", [128, 16, D], BF16)
            wq = sb(eb, "wq", [128, 8, 2048], BF16)
            skt = sb(eb, "skt", [128, 16, 128], BF16)
            bpb = sb(eb, "bpb", [128, 2048], F32)
            ppb = sb(eb, "ppb", [128, 136], F32)
            NW0 = 120
            S.dma("sp", lambda e: e.dma_start(out=bpb[:, :], in_=bpb_d.ap().partition_broadcast(128)), dst=bpb)
            S.dma("sp", lambda e: e.dma_start(out=ppb[:, :], in_=pp_d.ap()), dst=ppb)
            with ExitStack() as est:
                stg = [sb(est, "stgb%d" % i, [128, 2048], F32) for i in range(2)]
                n = 0
                for c in range(16):
                    st = stg[n % 2]
                    S.dma("sp", lambda e, st=st, c=c: e.dma_start(out=st[:, 0:D], in_=wout_d[c * 128:(c + 1) * 128, :]), dst=st)
                    eng = "act" if n % 2 == 0 else "dve"
                    if eng == "act":
                        S.op("act", lambda e, st=st, c=c: e.activation(out=wout[:, c, :], in_=st[:, 0:D], func=AF.Copy,
                                                                       scale=ppb[:, NW0 + c:NW0 + c + 1]), r=[st], w=[wout], c=[ppb])
                    else:
                        S.op("dve", lambda e, st=st, c=c: e.tensor_scalar(out=wout[:, c, :], in0=st[:, 0:D], scalar1=ppb[:, NW0 + c:NW0 + c + 1],
                                                                          scalar2=None, op0=ALU.mult), r=[st], w=[wout], c=[ppb])
                    n += 1
                for k in range(8):
                    st = stg[n % 2]
                    S.dma("sp", lambda e, st=st, k=k: e.dma_start(out=st[:, :], in_=wq_d[k * 128:(k + 1) * 128, :]), dst=st)
                    if n % 2 == 0:
                        S.op("act", lambda e, st=st, k=k: e.activation(out=wq[:, k, :], in_=st[:, :], func=AF.Copy), r=[st], w=[wq])
                    else:
                        S.op("dve", lambda e, st=st, k=k: e.tensor_copy(out=wq[:, k, :], in_=st[:, :]), r=[st], w=[wq])
                    n += 1
                st = stg[n % 2]
                S.dma("sp", lambda e, st=st: e.dma_start(out=st[:, :], in_=skt_d.ap()), dst=st)
                S.op("dve", lambda e, st=st: e.tensor_copy(out=skt[:, :, :].rearrange("p j k -> p (j k)"), in_=st[:, :]), r=[st], w=[skt])
                S.barrier()
            GF0, GL0 = 0, 1024
            ytl = [sb(eb, "ytl%d" % i, [128, 16, 128], BF16) for i in range(2)]
            xtb = [sb(eb, "xtb%d" % i, [128, D], F32) for i in range(2)]
            h1s = [sb(eb, "h1_%d" % i, [128, D], F32) for i in range(2)]
            xn2s = [sb(eb, "xn2_%d" % i, [128, D], BF16) for i in range(2)]
            idss = [sb(eb, "ids%d" % i, [128, NSLOT], I32) for i in range(2)]
            gatess = [sb(eb, "gates%d" % i, [128, NSLOT], F32) for i in range(2)]
            junkb = sb(eb, "junkb", [128, D], BF16)
            smb = sb(eb, "smb", [128, 16], F32)
            smo = sb(eb, "smo", [128, 4], F32)
            mhalf = sb(eb, "mhalf", [128, 1], F32)
            S.op("pool", lambda e: e.memset(mhalf[:, :], -0.5), w=[mhalf])
            n2T = sb(eb, "n2T", [128, 8, 128], BF16)
            qT = sb(eb, "qT", [128, 16, 128], BF16)
            ssb = sb(eb, "ssb", [128, 16, 128], F32)
            ss2 = sb(eb, "ss2", [128, 16, 128], F32)
            vv = sb(eb, "vv", [128, 16, 16], F32)
            ixu = sb(eb, "ixu", [128, 16, 16], U32)
            ixf = sb(eb, "ixf", [128, 16, 16], F32)
            cand = Tl(ssb.t[:, :, :].rearrange("p (h two) k -> p h (two k)", two=2), "cand_alias")
            cand.b = ssb.b
            cand2 = Tl(ss2.t[:, :, :].rearrange("p (h two) k -> p h (two k)", two=2), "cand2_alias")
            cand2.b = ss2.b
            best = sb(eb, "best", [128, 8, 16], F32)
            posu = sb(eb, "posu", [128, 8, 16], U32)
            pa = sb(eb, "pa", [128, 128], U32)
            pb = sb(eb, "pb", [128, 128], U32)
            paf = sb(eb, "paf", [128, 128], F32)
            pbf = sb(eb, "pbf", [128, 128], F32)
            oh = sb(eb, "oh", [128, 128, 16], F32)
            i1 = sb(eb, "i1", [128, 128], F32)
            i2 = sb(eb, "i2", [128, 128], F32)
            ex = sb(eb, "ex", [128, 8, 16], F32)
            hidr = [sb(eb, "hid%d" % i, [128, 4], F32) for i in range(6)]
            UVb = [sb(eb, "UVb%d" % i, [128, 2 * D], BF16) for i in range(NGB)]
            dg = [sb(eb, "dg%d" % i, [128, 128], BF16) for i in range(6)]
            ot = [sb(eb, "ot%d" % i, [128, D], F32) for i in range(2)]
            junkfs = [sb(eb, "junkf%d" % i, [128, D], BF16) for i in range(3)]

            vvB = [Buf("vv%d" % j) for j in range(16)]
            ixB = [Buf("ix%d" % j) for j in range(16)]
            s2B = [Buf("s2_%d" % j) for j in range(16)]
            beB = [Buf("be%d" % h) for h in range(8)]
            poB = [Buf("po%d" % h) for h in range(8)]
            c2B = [Buf("c2_%d" % h) for h in range(8)]

            def stageA(it):
                seq, ti = divmod(it, ntile)
                par = it % 2
                yl, xt, h1, xn2, ids, gates = ytl[par], xtb[par], h1s[par], xn2s[par], idss[par], gatess[par]
                S.dma("sp", lambda e: e.dma_start(out=yl[:, :, :].rearrange("p k t -> p (k t)"), in_=yt_d[seq * NT + ti, :, :]),
                      dst=yl, r=[yt_b])
                S.dma("sp", lambda e: e.dma_start(out=xt[:, :], in_=x_d[seq, ti * 128:(ti + 1) * 128, :]), dst=xt)
                yield
                for nb in range(2):
                    p = gpool.get()
                    for c in range(16):
                        S.op("pe", lambda e, p=p, c=c, nb=nb: e.matmul(p[:, :], lhsT=yl[:, c, :], rhs=wout[:, c, nb * 512:(nb + 1) * 512],
                                                                       start=(c == 0), stop=(c == 15)), r=[yl], w=[p], c=[wout])
                        if c % 2 == 1:
                            yield
                    S.op("dve", lambda e, p=p, nb=nb: e.tensor_tensor(out=h1[:, nb * 512:(nb + 1) * 512], in0=p[:, :],
                                                                      in1=xt[:, nb * 512:(nb + 1) * 512], op=ALU.add), r=[p, xt], w=[h1])
                    yield
                S.op("act", lambda e: e.activation(out=junkb[:, :], in_=h1[:, :], func=AF.Square, accum_out=smb[:, 0:1]), r=[h1], w=[smb, junkb])
                S.op("pool", lambda e: e.tensor_scalar(out=smb[:, 1:2], in0=smb[:, 0:1], scalar1=1.0 / D, scalar2=EPS, op0=ALU.mult, op1=ALU.add),
                     r=[smb], w=[smb])
                S.op("pool", lambda e: e.tensor_tensor(out=smb[:, 2:3], in0=smb[:, 1:2], in1=mhalf[:, 0:1], op=ALU.pow), r=[smb], w=[smb], c=[mhalf])
                yield
                S.op("dve", lambda e: e.scalar_tensor_tensor(out=xn2[:, :], in0=h1[:, :], scalar=smb[:, 2:3], in1=bpb[:, GF0:GF0 + D],
                                                             op0=ALU.mult, op1=ALU.mult), r=[h1, smb], w=[xn2], c=[bpb])
                yield
                pt = gpool.get()
                ptb = pt.t[:].bitcast(BF16)
                for k in range(8):
                    S.op("pe", lambda e, k=k: e.transpose(out=ptb[:, k * 128:(k + 1) * 128], in_=xn2[:, k * 128:(k + 1) * 128],
                                                          identity=identb[:, :]), r=[xn2], w=[pt], c=[identb])
                yield
                S.op("act", lambda e: e.activation(out=n2T[:, :, :].rearrange("p k t -> p (k t)"), in_=ptb[:, :], func=AF.Copy), r=[pt], w=[n2T])
                yield
                for qb in range(4):
                    p = gpool.get()
                    for j in range(4):
                        jj = qb * 4 + j
                        for k in range(8):
                            S.op("pe", lambda e, p=p, j=j, jj=jj, k=k: e.matmul(
                                p[:, j * 128:(j + 1) * 128], lhsT=wq[:, k, jj * 128:(jj + 1) * 128], rhs=n2T[:, k, :],
                                start=(k == 0), stop=(k == 7)), r=[n2T], w=[p], c=[wq])
                        yield
                    S.op("act", lambda e, p=p, qb=qb: e.activation(out=qT[:, 4 * qb:4 * qb + 4, :].rearrange("p j t -> p (j t)"), in_=p[:, :],
                                                                   func=AF.Copy), r=[p], w=[qT])
                    yield
                for qb in range(4):
                    p = gpool.get()
                    for j in range(4):
                        jj = qb * 4 + j
                        S.op("pe", lambda e, p=p, j=j, jj=jj: e.matmul(p[:, j * 128:(j + 1) * 128], lhsT=qT[:, jj, :], rhs=skt[:, jj, :],
                                                                       start=True, stop=True), r=[qT], w=[p], c=[skt])
                    yield
                    S.op("act", lambda e, p=p, qb=qb: e.activation(out=ssb[:, 4 * qb:4 * qb + 4, :].rearrange("p j t -> p (j t)"), in_=p[:, :],
                                                                   func=AF.Copy), r=[p], w=[ssb])
                    yield
                def topk_steps(vals, vals2, vB, iB, v2B, outv, outi, idxs, alias_w=lambda j: []):
                    steps = []
                    for j in idxs:
                        steps.append(lambda j=j: S.op("dve", lambda e: e.max(out=outv[:, j, 0:8], in_=vals[:, j, :]), r=[vals], w=[vB[j]]))
                    for j in idxs:
                        steps.append(lambda j=j: S.op("dve", lambda e: e.max_index(out=outi[:, j, 0:8], in_max=outv[:, j, 0:8], in_values=vals[:, j, :]),
                                                      r=[vals, vB[j]], w=[iB[j]]))
                    for j in idxs:
                        steps.append(lambda j=j: S.op("dve", lambda e: e.match_replace(out=vals2[:, j, :], in_to_replace=outv[:, j, 0:8],
                                                                                        in_values=vals[:, j, :], imm_value=-1e30),
                                                      r=[vals, vB[j]], w=[v2B[j]] + alias_w(j)))
                    for j in idxs:
                        steps.append(lambda j=j: S.op("dve", lambda e: e.max(out=outv[:, j, 8:16], in_=vals2[:, j, :]), r=[v2B[j]], w=[vB[j]]))
                    for j in idxs:
                        steps.append(lambda j=j: S.op("dve", lambda e: e.max_index(out=outi[:, j, 8:16], in_max=outv[:, j, 8:16], in_values=vals2[:, j, :]),
                                                      r=[v2B[j], vB[j]], w=[iB[j]]))
                    return steps

                for j0 in range(0, 16, 4):
                    st = topk_steps(ssb, ss2, vvB, ixB, s2B, vv, ixu, range(j0, j0 + 4), alias_w=lambda j: [c2B[j // 2]])
                    for i_, f_ in enumerate(st):
                        f_()
                        if i_ % 3 == 2:
                            yield
                    yield
                S.op("dve", lambda e: e.tensor_copy(out=ixf[:, :, :], in_=ixu[:, :, :]), r=ixB, w=[ixf])
                v4 = vv[:, :, :].rearrange("p (h two) k -> p h two k", two=2)
                S.op("dve", lambda e: e.tensor_tensor(
                    out=cand[:, :, :].rearrange("p h (a b) -> p h a b", a=16),
                    in0=v4[:, :, 0, :].unsqueeze(3).to_broadcast([128, 8, 16, 16]),
                    in1=v4[:, :, 1, :].unsqueeze(2).to_broadcast([128, 8, 16, 16]), op=ALU.add), r=vvB, w=[cand])
                yield
                for h0 in range(0, 8, 4):
                    st = topk_steps(cand, cand2, beB, poB, c2B, best, posu, range(h0, h0 + 4), alias_w=lambda h: [s2B[2 * h], s2B[2 * h + 1]])
                    for i_, f_ in enumerate(st):
                        f_()
                        if i_ % 3 == 2:
                            yield
                    yield
                S.op("dve", lambda e: e.tensor_tensor(out=ex[:, :, :], in0=best[:, :, :], in1=best[:, :, 0:1].to_broadcast([128, 8, 16]),
                                                      op=ALU.subtract), r=beB, w=[ex])
                yield
                S.op("act", lambda e: e.activation(out=ex[:, :, :], in_=ex[:, :, :], func=AF.Exp), r=[ex], w=[ex])
                yield
                S.op("dve", lambda e: e.tensor_reduce(out=smb[:, 4:12], in_=ex[:, :, :], axis=AX.X, op=ALU.add), r=[ex], w=[smb])
                S.op("dve", lambda e: e.reciprocal(out=smb[:, 4:12], in_=smb[:, 4:12]), r=[smb], w=[smb])
                S.op("dve", lambda e: e.tensor_tensor(out=gates[:, :].rearrange("p (h k) -> p h k", h=8), in0=ex[:, :, :],
                                                      in1=smb[:, 4:12].unsqueeze(2).to_broadcast([128, 8, 16]), op=ALU.mult),
                     r=[ex, smb], w=[gates])
                yield
                pos2 = posu[:, :, :].rearrange("p h k -> p (h k)")
                S.op("dve", lambda e: e.tensor_single_scalar(out=pa[:, :], in_=pos2, scalar=4, op=ALU.logical_shift_right), r=poB, w=[pa])
                S.op("dve", lambda e: e.tensor_single_scalar(out=pb[:, :], in_=pos2, scalar=15, op=ALU.bitwise_and), r=poB, w=[pb])
                S.op("dve", lambda e: e.tensor_copy(out=paf[:, :], in_=pa[:, :]), r=[pa], w=[paf])
                S.op("dve", lambda e: e.tensor_copy(out=pbf[:, :], in_=pb[:, :]), r=[pb], w=[pbf])
                yield
                ix4 = ixf[:, :, :].rearrange("p (h two) k -> p h two k", two=2)
                for half, (pf, idst) in enumerate([(paf, i1), (pbf, i2)]):
                    S.op("dve", lambda e, pf=pf: e.tensor_tensor(
                        out=oh[:, :, :], in0=cst[:, IOTA0:IOTA0 + 16].unsqueeze(1).to_broadcast([128, 128, 16]),
                        in1=pf[:, :].unsqueeze(2).to_broadcast([128, 128, 16]), op=ALU.is_equal), r=[pf], w=[oh], c=[cst])
                    yield
                    S.op("dve", lambda e, half=half: e.tensor_tensor(
                        out=oh[:, :, :].rearrange("p (h k) a -> p h k a", h=8), in0=oh[:, :, :].rearrange("p (h k) a -> p h k a", h=8),
                        in1=ix4[:, :, half, :].unsqueeze(2).to_broadcast([128, 8, 16, 16]), op=ALU.mult), r=[oh, ixf], w=[oh])
                    yield
                    S.op("dve", lambda e, idst=idst: e.tensor_reduce(out=idst[:, :], in_=oh[:, :, :], axis=AX.X, op=ALU.add), r=[oh], w=[idst])
                    yield
                S.op("dve", lambda e: e.scalar_tensor_tensor(out=i1[:, :], in0=i1[:, :], scalar=128.0, in1=i2[:, :], op0=ALU.mult, op1=ALU.add),
                     r=[i1, i2], w=[i1])
                S.op("dve", lambda e: e.tensor_copy(out=ids[:, :], in_=i1[:, :]), r=[i1], w=[ids])
                yield

            def stageB(it, agen):
                seq, ti = divmod(it, ntile)
                par = it % 2
                h1, xn2, ids, gates = h1s[par], xn2s[par], idss[par], gatess[par]
                o = ot[par]
                h2 = o
                ffnb = ffnbs[par]

                def pump(n=1):
                    if agen is not None:
                        for _ in range(n):
                            try:
                                next(agen)
                            except StopIteration:
                                return

                for k in range(NSLOT):
                    uvb = UVb[k % NGB]
                    hk = hidr[k % 6]
                    d = dg[k % 6]
                    S.dma("pool", lambda e, uvb=uvb, k=k: e.indirect_dma_start(
                        out=uvb[:, :], out_offset=None, in_=uv_d[:, :],
                        in_offset=bass.IndirectOffsetOnAxis(ap=ids[:, k:k + 1], axis=0)), dst=uvb, r=[ids], c=uv_bufs)
                    jf = junkfs[k % 3]
                    S.op("dve", lambda e, uvb=uvb, hk=hk, jf=jf: e.scalar_tensor_tensor(
                        out=jf[:, :], in0=uvb[:, 0:D], scalar=1.0, in1=xn2[:, :], op0=ALU.mult, op1=ALU.mult,
                        accum_out=hk[:, 0:1]), r=[uvb, xn2], w=[hk, jf])
                    S.op("act", lambda e, hk=hk: e.activation(out=hk[:, 1:2], in_=hk[:, 0:1], func=AF.Gelu), r=[hk], w=[hk])
                    S.op("act", lambda e, hk=hk, k=k: e.activation(out=hk[:, 2:3], in_=hk[:, 1:2], func=AF.Copy, scale=gates[:, k:k + 1]),
                         r=[hk, gates], w=[hk])
                    S.op("act", lambda e, d=d, hk=hk: e.activation(out=d[:, :], in_=cst[:, 0:128], func=AF.Copy, scale=hk[:, 2:3]),
                         r=[hk], w=[d], c=[cst])
                    for nb in range(2):
                        S.op("pe", lambda e, d=d, uvb=uvb, nb=nb, k=k: e.matmul(
                            ffnb[nb][:, :], lhsT=d[:, :], rhs=uvb[:, D + nb * 512:D + (nb + 1) * 512], start=(k == 0), stop=(k == NSLOT - 1)),
                            r=[d, uvb], w=[ffnb[nb]])
                    pump(1)
                pump(100000)
                for nb in range(2):
                    S.op("dve", lambda e, nb=nb: e.tensor_tensor(out=h2[:, nb * 512:(nb + 1) * 512], in0=ffnb[nb][:, :],
                                                                 in1=h1[:, nb * 512:(nb + 1) * 512], op=ALU.add), r=[ffnb[nb], h1], w=[h2])
                S.op("act", lambda e: e.activation(out=junkb[:, :], in_=h2[:, :], func=AF.Square, accum_out=smo[:, 0:1]), r=[h2], w=[smo, junkb])
                S.op("pool", lambda e: e.tensor_scalar(out=smo[:, 1:2], in0=smo[:, 0:1], scalar1=1.0 / D, scalar2=EPS, op0=ALU.mult, op1=ALU.add),
                     r=[smo], w=[smo])
                S.op("pool", lambda e: e.tensor_tensor(out=smo[:, 2:3], in0=smo[:, 1:2], in1=mhalf[:, 0:1], op=ALU.pow), r=[smo], w=[smo], c=[mhalf])
                S.op("dve", lambda e: e.scalar_tensor_tensor(out=o[:, :], in0=h2[:, :], scalar=smo[:, 2:3], in1=bpb[:, GL0:GL0 + D],
                                                             op0=ALU.mult, op1=ALU.mult), r=[h2, smo], w=[o], c=[bpb])
                S.dma("sp", lambda e: e.dma_start(out=out_d[seq, ti * 128:(ti + 1) * 128, :], in_=o[:, :]), dst=out_b, r=[o])

            ntot = nseq * ntile
            g0 = stageA(0)
            for _ in g0:
                pass
            for it in range(ntot):
                agen = stageA(it + 1) if it + 1 < ntot else None
                stageB(it, agen)
            S.final_wait("sp", [out_b])
    return nc


def _host_inputs(inputs):
    f = lambda a: np.ascontiguousarray(np.asarray(a, dtype=np.float32))
    x = f(inputs["x"])
    pp = np.concatenate([
        f(inputs["g_mix"])[0].reshape(8, 128).T,
        f(inputs["conv_ssd_w"])[0].T.reshape(20, 128, 4).transpose(1, 0, 2).reshape(128, 80),
        f(inputs["conv_ssd_b"])[0].reshape(20, 128).T,
        f(inputs["conv_sc_w"])[0].T.reshape(4, 128, 3).transpose(1, 0, 2).reshape(128, 12),
        np.concatenate([f(inputs["ssd_norm_w"])[0], np.ones(512, np.float32)]).reshape(16, 128).T,
    ], axis=1)
    bpa = np.concatenate([f(inputs["dt_bias"])[0], f(inputs["a_log"])[0], f(inputs["d_skip"])[0]])[None, :]
    bpb = np.concatenate([f(inputs["g_ffn"])[0], f(inputs["g_final"])])[None, :]
    skt = f(inputs["sub_keys"])[0].transpose(3, 0, 1, 2).reshape(128, 2048)
    r = np.arange(128)
    cst = np.concatenate([
        np.eye(128, dtype=np.float32),
        (r[:, None] <= r[None, :]).astype(np.float32),
        np.ones((128, 128), np.float32),
        np.where(r[None, :] < r[:, None], np.float32(NEG), np.float32(0.0)).astype(np.float32),
        np.broadcast_to(np.arange(16, dtype=np.float32)[None, :], (128, 16)),
    ], axis=1)
    shared = {
        "meta": f(inputs["meta_tokens"]), "w_in": f(inputs["w_in"])[0], "w_out": f(inputs["w_out"])[0],
        "w_q": f(inputs["w_q"])[0], "skt": np.ascontiguousarray(skt), "expert_u": f(inputs["expert_u"])[0],
        "expert_v": f(inputs["expert_v"])[0], "pp": np.ascontiguousarray(pp), "bpa": np.ascontiguousarray(bpa),
        "bpb": np.ascontiguousarray(bpb), "cst": np.ascontiguousarray(cst),
    }
    return x, shared


def kernel(**inputs):
    x, shared = _host_inputs(inputs)
    ncores = 8
    nc = build()
    in_maps = []
    for c in range(ncores):
        m = dict(shared)
        m["x"] = np.ascontiguousarray(x[c * NSEQ:(c + 1) * NSEQ])
        in_maps.append(m)
    res = run_bass_kernel_spmd(nc, in_maps, core_ids=list(range(ncores)))
    out = np.concatenate([np.asarray(r["out"], dtype=np.float32) for r in res.results], axis=0)
    return out
```

```python
import numpy as np
from contextlib import ExitStack
import concourse.bass as bass
import concourse.mybir as mybir
from concourse.bass_utils import run_bass_kernel_spmd

F32 = mybir.dt.float32
BF16 = mybir.dt.bfloat16
I32 = mybir.dt.int32
U32 = mybir.dt.uint32
AF = mybir.ActivationFunctionType
ALU = mybir.AluOpType
AX = mybir.AxisListType

D = 1024
SEQ = 2048
NSEQ = 2
NT = SEQ // 128
NMETA = 16
DIN = 5656
NH = 24
HP = 64
NG = 4
DSSD = 1536
EPS = 1e-6
C_Z, C_XBC, C_DT, C_SCB, C_SCC, C_SCX = 0, 1536, 4096, 4120, 4632, 5144
NSLOT = 128
NEXP = 16384
NGB = 10
NEG = -30000.0
STRICT_SAME_ENGINE = True


class Buf:
    __slots__ = ("name", "w", "r", "dsem", "dcnt")

    def __init__(self, name):
        self.name = name
        self.w = None
        self.r = []
        self.dsem = None
        self.dcnt = 0


class Tl:
    def __init__(self, t, name):
        self.t = t
        self.b = Buf(name)

    def __getitem__(self, k):
        return self.t[k]


def _b(x):
    return x.b if isinstance(x, Tl) else x


class Sched:
    def __init__(self, nc, es):
        self.nc = nc
        self.es = es
        self.eng = {"pe": nc.tensor, "act": nc.scalar, "dve": nc.vector, "pool": nc.gpsimd, "sp": nc.sync}
        self.sem = {k: es.enter_context(nc.semaphore("s_" + k)) for k in self.eng}
        self.cnt = {k: 0 for k in self.eng}
        self.seen = {k: {} for k in self.eng}
        self.dbufs = []
        self.nsem = 0

    def _wait(self, e, deps):
        best = {}
        for (s, v) in deps:
            key = id(s)
            if key not in best or v > best[key][1]:
                best[key] = (s, v)
        for key, (s, v) in best.items():
            if self.seen[e].get(key, 0) >= v:
                continue
            self.eng[e].wait_ge(s, v)
            self.seen[e][key] = v

    def _deps(self, r, w, c, own=None):
        deps = []
        for b in r:
            if b.w is not None:
                deps.append(b.w)
        for b in c:
            if b.w is not None:
                deps.append(b.w)
        for b in w:
            if b.w is not None and (STRICT_SAME_ENGINE or b.w[0] is not own):
                deps.append(b.w)
            deps.extend(x for x in b.r if STRICT_SAME_ENGINE or x[0] is not own)
        return deps

    def op(self, e, fn, r=(), w=(), c=()):
        r = [_b(x) for x in r]
        w = [_b(x) for x in w]
        c = [_b(x) for x in c]
        deps = self._deps(r, w, c, own=self.sem[e])
        if e == "pe":
            deps = [d for d in deps if d[0] is not self.sem["pe"]]
        self._wait(e, deps)
        inst = fn(self.eng[e])
        self.cnt[e] += 1
        inst.then_inc(self.sem[e], 1)
        t = (self.sem[e], self.cnt[e])
        for b in w:
            b.w = t
            b.r = []
        for b in r:
            b.r.append(t)
        return inst

    def dma(self, q, fn, dst, r=(), c=()):
        dst = _b(dst)
        r = [_b(x) for x in r]
        c = [_b(x) for x in c]
        deps = self._deps(r, [dst], c)
        self._wait(q, deps)
        if dst.dsem is None:
            dst.dsem = self.es.enter_context(self.nc.semaphore("d%d_%s" % (self.nsem, dst.name)))
            self.nsem += 1
            self.dbufs.append(dst)
        inst = fn(self.eng[q])
        dst.dcnt += 16
        inst.then_inc(dst.dsem, 16)
        t = (dst.dsem, dst.dcnt)
        dst.w = t
        dst.r = []
        for b in r:
            b.r.append(t)
        return inst

    def barrier(self):
        allv = [(self.sem[k], self.cnt[k]) for k in self.eng if self.cnt[k] > 0]
        allv += [(b.dsem, b.dcnt) for b in self.dbufs]
        for e in self.eng:
            self._wait(e, [d for d in allv if d[0] is not self.sem[e]])

    def final_wait(self, e, bufs):
        self._wait(e, [(_b(b).dsem, _b(b).dcnt) for b in bufs])


class Pool:
    def __init__(self, tiles):
        self.tiles = tiles
        self.i = 0

    def get(self):
        t = self.tiles[self.i % len(self.tiles)]
        self.i += 1
        return t


def build(debug=None, nseq=NSEQ, ntile=NT):
    nc = bass.Bass("TRN2", target_bir_lowering=False)
    dbgA = debug == "A"
    x_d = nc.dram_tensor("x", [NSEQ, SEQ, D], F32, kind="ExternalInput")
    meta_d = nc.dram_tensor("meta", [NMETA, D], F32, kind="ExternalInput")
    win_d = nc.dram_tensor("w_in", [D, DIN], F32, kind="ExternalInput")
    wout_d = nc.dram_tensor("w_out", [2048, D], F32, kind="ExternalInput")
    wq_d = nc.dram_tensor("w_q", [D, 2048], F32, kind="ExternalInput")
    skt_d = nc.dram_tensor("skt", [128, 2048], F32, kind="ExternalInput")
    eu_d = nc.dram_tensor("expert_u", [NEXP, D], F32, kind="ExternalInput")
    ev_d = nc.dram_tensor("expert_v", [NEXP, D], F32, kind="ExternalInput")
    pp_d = nc.dram_tensor("pp", [128, 136], F32, kind="ExternalInput")
    bpa_d = nc.dram_tensor("bpa", [1, 72], F32, kind="ExternalInput")
    bpb_d = nc.dram_tensor("bpb", [1, 2048], F32, kind="ExternalInput")
    cst_d = nc.dram_tensor("cst", [128, 528], F32, kind="ExternalInput")
    out_d = nc.dram_tensor("out", [NSEQ, SEQ, D], F32, kind="ExternalOutput")
    yt_d = nc.dram_tensor("yt_scr", [NSEQ * NT, 128, 2048], BF16,
                          **({"kind": "ExternalOutput"} if dbgA else {}))

    uv_d = nc.dram_tensor("uv_scr", [NEXP, 2 * D], BF16)
    dbg_d = {}
    if dbgA:
        for nm, shp, dt_ in [("dts", [128, 192], F32), ("xsB", [128, 2048], BF16), ("xdt", [128, 1536], BF16),
                             ("cbT", [128, 512], F32), ("t1", [128, 1536], F32), ("yb", [128, 1536], F32),
                             ("St", [128, 1536], F32), ("Smeta", [128, 1536], F32), ("sz", [128, 1536], BF16),
                             ("Dm1", [128, 384], F32), ("MT1", [128, 384], BF16), ("xact", [128, 2560], BF16)]:
            dbg_d[nm] = nc.dram_tensor("dbg_" + nm, shp, dt_, kind="ExternalOutput")
    dbg_bufs = []
    with ExitStack() as es:
        S = Sched(nc, es)

        def sb(stack, name, shape, dt):
            return Tl(stack.enter_context(nc.sbuf_tensor("sb_" + name, shape, dt)), name)

        ps = [Tl(es.enter_context(nc.psum_tensor("ps%d" % i, [128, 512], F32)), "ps%d" % i) for i in range(8)]
        yt_b = Buf("yt_scr")
        out_b = Buf("out")

        cst = sb(es, "cst", [128, 528], F32)
        identb = sb(es, "identb", [128, 128], BF16)
        S.dma("sp", lambda e: e.dma_start(out=cst[:, :], in_=cst_d.ap()), dst=cst)
        S.op("dve", lambda e: e.tensor_copy(out=identb[:, :], in_=cst[:, 0:128]), r=[cst], w=[identb])
        ident = cst
        TRIU0, ONES0, NEGM0, IOTA0 = 128, 256, 384, 512
        uv_bufs = [Buf("uv%d" % i) for i in range(4)]
        UVCH = 1024
        uv_jobs = [(r0, half) for r0 in range(0, NEXP, UVCH) for half in range(2)]
        uv_state = {"i": 0}

        def uv_issue(n):
            for _ in range(n):
                i = uv_state["i"]
                if dbgA or i >= len(uv_jobs):
                    return
                r0, half = uv_jobs[i]
                src_d = eu_d if half == 0 else ev_d
                S.dma("pool", lambda e: e.dma_start(out=uv_d[r0:r0 + UVCH, half * D:(half + 1) * D], in_=src_d[r0:r0 + UVCH, :]),
                      dst=uv_bufs[i % 4])
                uv_state["i"] = i + 1

        with ExitStack() as ea:
            fpool = Pool(ps[0:2])
            bpool = Pool(ps[2:5])
            win = sb(ea, "win", [128, 8, DIN], BF16)
            pp = sb(ea, "pp", [128, 136], F32)
            bpa = sb(ea, "bpa", [128, 72], F32)
            A_bc = sb(ea, "A_bc", [128, NH], F32)
            S.dma("sp", lambda e: e.dma_start(out=pp[:, :], in_=pp_d.ap()), dst=pp)
            S.dma("sp", lambda e: e.dma_start(out=bpa[:, :], in_=bpa_d.ap().partition_broadcast(128)), dst=bpa)
            GM0, CW0, CB0, SCW0, NW0 = 0, 8, 88, 108, 120
            DTB0, AL0, DSK0 = 0, 24, 48
            S.op("act", lambda e: e.activation(out=A_bc[:, :], in_=bpa[:, AL0:AL0 + NH], func=AF.Exp), r=[bpa], w=[A_bc])
            S.op("dve", lambda e: e.tensor_scalar(out=A_bc[:, :], in0=A_bc[:, :], scalar1=-1.0, scalar2=None, op0=ALU.mult),
                 r=[A_bc], w=[A_bc])
            with ExitStack() as est:
                stg = [sb(est, "stg%d" % i, [128, 2828], F32) for i in range(2)]
                n = 0
                for k in range(8):
                    for hb in range(2):
                        st = stg[n % 2]
                        S.dma("sp", lambda e, st=st, k=k, hb=hb: e.dma_start(
                            out=st[:, :], in_=win_d[k * 128:(k + 1) * 128, hb * 2828:(hb + 1) * 2828]), dst=st)
                        if n % 2 == 0:
                            S.op("act", lambda e, st=st, k=k, hb=hb: e.activation(
                                out=win[:, k, hb * 2828:(hb + 1) * 2828], in_=st[:, :], func=AF.Copy,
                                scale=pp[:, GM0 + k:GM0 + k + 1]), r=[st], w=[win], c=[pp])
                        else:
                            S.op("dve", lambda e, st=st, k=k, hb=hb: e.tensor_scalar(
                                out=win[:, k, hb * 2828:(hb + 1) * 2828], in0=st[:, :],
                                scalar1=pp[:, GM0 + k:GM0 + k + 1], scalar2=None, op0=ALU.mult), r=[st], w=[win], c=[pp])
                        n += 1
                S.barrier()
            xts = [sb(ea, "xt%d" % i, [128, D], F32) for i in range(1)]
            xn = sb(ea, "xn", [128, D], BF16)
            junk2 = sb(ea, "junk2", [128, 384], BF16)
            sm = sb(ea, "sm", [128, 4], F32)
            smk = sb(ea, "smk", [128, 16], F32)
            nT = sb(ea, "nT", [128, 8, 128], BF16)
            xpres = [sb(ea, "xpre%d" % i, [128, 20, 131], BF16) for i in range(2)]
            hxs = [sb(ea, "hx%d" % i, [128, 20, 3], BF16) for i in range(3)]
            hcs = [sb(ea, "hc%d" % i, [128, 4, 2], F32) for i in range(3)]
            xacts = [sb(ea, "xact%d" % i, [128, 20, 128], BF16) for i in range(2)]
            cacc = [sb(ea, "cacc%d" % i, [128, 128], F32) for i in range(8)]
            scbs = [sb(ea, "scb%d" % i, [128, 4, 128], BF16) for i in range(2)]
            scc = sb(ea, "scc", [128, 4, 128], F32)
            cxs = [sb(ea, "cx%d" % i, [128, 4, 130], F32) for i in range(2)]
            sacc = sb(ea, "sacc", [128, 4, 128], F32)
            szs = [sb(ea, "sz%d" % i, [128, DSSD], BF16) for i in range(3)]
            xsB = sb(ea, "xsB", [128, 2048], BF16)
            xdt = sb(ea, "xdt", [128, DSSD], BF16)
            xdd = sb(ea, "xdd", [128, DSSD], BF16)
            dtfs = [sb(ea, "dtf%d" % i, [128, 3, NH], F32) for i in range(3)]
            dts = sb(ea, "dts", [128, 8, NH], F32)
            rhsA = [sb(ea, "rhsA%d" % i, [128, 384], F32) for i in range(2)]
            Dm = [sb(ea, "Dm%d" % i, [128, 384], F32) for i in range(2)]
            MT = [sb(ea, "MT%d" % i, [128, 384], BF16) for i in range(3)]
            cbT = sb(ea, "cbT", [128, 512], F32)
            t1 = sb(ea, "t1", [128, DSSD], F32)
            yn = sb(ea, "yn", [128, DSSD], BF16)
            YTs = [sb(ea, "YT%d" % i, [128, 16, 128], BF16) for i in range(2)]
            St = sb(ea, "St", [128, DSSD], F32)
            Stmp = sb(ea, "Stmp", [128, DSSD], F32)
            t1b = sb(ea, "t1b", [128, DSSD], BF16)
            yb = t1
            Sbf = sb(ea, "Sbf", [128, DSSD], BF16)
            Smeta = sb(ea, "Smeta", [128, DSSD], F32)
            halo_x = sb(ea, "halo_x", [128, 20, 3], F32)
            halo_c = sb(ea, "halo_c", [128, 4, 2], F32)
            state = {"n": 0}

            def frontA(seq, ti, T, meta, par, sj=0, jg=0):
                first = (ti == 0) and not meta
                last = (ti == ntile - 1) and not meta
                xpre = xpres[jg % 2]
                cx = cxs[jg % 2]
                scb = scbs[jg % 2]
                junk = xn
                xt = xts[0]
                YT = YTs[par]
                xact = xacts[par]
                sz = szs[sj]
                dtf = dtfs[sj]
                pool = fpool
                src = meta_d.ap() if meta else x_d[seq, ti * 128:(ti + 1) * 128, :]
                S.dma("sp", lambda e: e.dma_start(out=xt[0:T, :], in_=src), dst=xt)
                S.op("act", lambda e: e.activation(out=junk[0:T, 0:D], in_=xt[0:T, :], func=AF.Square,
                                                   accum_out=sm[0:T, 0:1]), r=[xt], w=[xn, sm])
                S.op("act", lambda e: e.activation(out=sm[0:T, 1:2], in_=sm[0:T, 0:1], func=AF.Ln, scale=1.0 / D, bias=EPS),
                     r=[sm], w=[sm])
                S.op("act", lambda e: e.activation(out=sm[0:T, 2:3], in_=sm[0:T, 1:2], func=AF.Exp, scale=-0.5),
                     r=[sm], w=[sm])
                S.op("act", lambda e: e.activation(out=xn[0:T, :], in_=xt[0:T, :], func=AF.Copy, scale=sm[0:T, 2:3]),
                     r=[xt, sm], w=[xn])
                yield
                pt = pool.get()
                ptb = pt.t[:].bitcast(BF16)
                for k in range(8):
                    S.op("pe", lambda e, k=k: e.transpose(out=ptb[:, k * 128:k * 128 + T], in_=xn[0:T, k * 128:(k + 1) * 128],
                                                          identity=identb[0:T, 0:T]), r=[xn], w=[pt], c=[identb])
                yield
                S.op("dve", lambda e: e.tensor_copy(out=nT[:, :, 0:T], in_=ptb.rearrange("p (k t) -> p k t", k=8)[:, :, 0:T]),
                     r=[pt], w=[nT])
                yield

                fmres = {}

                def fm_bank(col0, nchunk=4):
                    p = pool.get()
                    fmres["p"] = p
                    for j in range(nchunk):
                        for k in range(8):
                            S.op("pe", lambda e, j=j, k=k: e.matmul(
                                p[:, j * 128:j * 128 + T], lhsT=win[:, k, col0 + j * 128:col0 + (j + 1) * 128],
                                rhs=nT[:, k, 0:T], start=(k == 0), stop=(k == 7)), r=[nT], w=[p], c=[win])
                        yield

                def p3(p):
                    return p.t[:].rearrange("p (j t) -> p j t", j=4)[:, :, 0:T]

                for bq in range(5):
                    yield from fm_bank(C_XBC + bq * 512)
                    p = fmres["p"]
                    eng = "act" if bq % 2 == 0 else "dve"
                    if eng == "act":
                        S.op("act", lambda e, p=p, bq=bq: e.activation(out=xpre[:, 4 * bq:4 * bq + 4, 3:3 + T], in_=p3(p), func=AF.Copy),
                             r=[p], w=[xpre])
                    else:
                        S.op("dve", lambda e, p=p, bq=bq: e.tensor_copy(out=xpre[:, 4 * bq:4 * bq + 4, 3:3 + T], in_=p3(p)),
                             r=[p], w=[xpre])
                    yield
                if not meta:
                    yield from fm_bank(C_SCB)
                    p = fmres["p"]
                    S.op("act", lambda e, p=p: e.activation(out=scb[:, :, 0:T], in_=p3(p), func=AF.Copy), r=[p], w=[scb])
                yield from fm_bank(C_SCC)
                p = fmres["p"]
                S.op("act", lambda e, p=p: e.activation(out=scc[:, :, 0:T], in_=p3(p), func=AF.Copy), r=[p], w=[scc])
                yield from fm_bank(C_SCX)
                p = fmres["p"]
                S.op("dve", lambda e, p=p: e.tensor_tensor(out=cx[:, :, 2:2 + T], in0=scc[:, :, 0:T], in1=p3(p), op=ALU.mult),
                     r=[p, scc], w=[cx])
                yield
                p = pool.get()
                for k in range(8):
                    S.op("pe", lambda e, k=k, p=p: e.matmul(p[0:T, 0:NH], lhsT=nT[:, k, 0:T], rhs=win[:, k, C_DT:C_DT + NH],
                                                            start=(k == 0), stop=(k == 7)), r=[nT], w=[p], c=[win])
                S.op("dve", lambda e, p=p: e.tensor_tensor(out=dtf[0:T, 2, :], in0=p[0:T, 0:NH], in1=bpa[0:T, DTB0:DTB0 + NH], op=ALU.add),
                     r=[p], w=[dtf], c=[bpa])
                S.op("act", lambda e: e.activation(out=dtf[0:T, 2, :], in_=dtf[0:T, 2, :], func=AF.Exp), r=[dtf], w=[dtf])
                S.op("act", lambda e: e.activation(out=dtf[0:T, 0, :], in_=dtf[0:T, 2, :], func=AF.Ln, bias=1.0), r=[dtf], w=[dtf])
                S.op("dve", lambda e: e.tensor_tensor(out=dtf[0:T, 1, :], in0=dtf[0:T, 0, :], in1=A_bc[0:T, :], op=ALU.mult),
                     r=[dtf], w=[dtf], c=[A_bc])
                yield
                if not meta:
                    for zb in range(3):
                        p = pool.get()
                        for k in range(8):
                            S.op("pe", lambda e, k=k, p=p, zb=zb: e.matmul(
                                p[0:T, :], lhsT=nT[:, k, 0:T], rhs=win[:, k, C_Z + zb * 512:C_Z + (zb + 1) * 512],
                                start=(k == 0), stop=(k == 7)), r=[nT], w=[p], c=[win])
                            if k % 4 == 3:
                                yield
                        S.op("act", lambda e, p=p, zb=zb: e.activation(out=sz[0:T, zb * 512:(zb + 1) * 512], in_=p[0:T, :], func=AF.Silu),
                             r=[p], w=[sz])
                        yield
                if not meta and not last:
                    S.op("act", lambda e: e.activation(out=hxs[sj][:, :, :], in_=xpre[:, :, T:T + 3], func=AF.Copy), r=[xpre], w=[hxs[sj]])
                    S.op("act", lambda e: e.activation(out=hcs[sj][:, :, :], in_=cx[:, :, T:T + 2], func=AF.Copy), r=[cx], w=[hcs[sj]])
                yield "mid"
                if meta:
                    S.op("pool", lambda e: e.memset(xpre[:, :, 0:3], 0.0), w=[xpre])
                    S.op("pool", lambda e: e.memset(cx[:, :, 0:2], 0.0), w=[cx])
                elif first:
                    S.op("act", lambda e: e.activation(out=xpre[:, :, 0:3], in_=halo_x[:, :, :], func=AF.Copy), r=[halo_x], w=[xpre])
                    S.op("act", lambda e: e.activation(out=cx[:, :, 0:2], in_=halo_c[:, :, :], func=AF.Copy), r=[halo_c], w=[cx])
                else:
                    hp = (sj + 2) % 3
                    S.op("act", lambda e: e.activation(out=xpre[:, :, 0:3], in_=hxs[hp][:, :, :], func=AF.Copy), r=[hxs[hp]], w=[xpre])
                    S.op("act", lambda e: e.activation(out=cx[:, :, 0:2], in_=hcs[hp][:, :, :], func=AF.Copy), r=[hcs[hp]], w=[cx])
                def conv_head(g):
                    for j in range(4):
                        k = 4 * g + j
                        ca = cacc[k % 8]
                        S.op("act", lambda e, k=k, ca=ca: e.activation(
                            out=ca[:, 0:T], in_=xpre[:, k, 3:3 + T], func=AF.Identity,
                            scale=pp[:, CW0 + 4 * k + 3:CW0 + 4 * k + 4], bias=pp[:, CB0 + k:CB0 + k + 1]), r=[xpre], w=[ca], c=[pp])

                def conv_taps(g):
                    for w_ in range(3):
                        for j in range(4):
                            k = 4 * g + j
                            ca = cacc[k % 8]
                            S.op("dve", lambda e, k=k, ca=ca, w_=w_: e.scalar_tensor_tensor(
                                out=ca[:, 0:T], in0=xpre[:, k, w_:w_ + T], scalar=pp[:, CW0 + 4 * k + w_:CW0 + 4 * k + w_ + 1],
                                in1=ca[:, 0:T], op0=ALU.mult, op1=ALU.add), r=[xpre, ca], w=[ca], c=[pp])

                def conv_tail(g):
                    for j in range(4):
                        k = 4 * g + j
                        ca = cacc[k % 8]
                        S.op("act", lambda e, k=k, ca=ca: e.activation(out=xact[:, k, 0:T], in_=ca[:, 0:T], func=AF.Silu), r=[ca], w=[xact])

                conv_head(0)
                yield
                for g in range(5):
                    if g + 1 < 5:
                        conv_head(g + 1)
                    conv_taps(g)
                    yield
                    conv_tail(g)
                    yield
                for k in range(4):
                    S.op("act", lambda e, k=k: e.activation(
                        out=sacc[:, k, 0:T], in_=cx[:, k, 2:2 + T], func=AF.Copy, scale=pp[:, SCW0 + 3 * k + 2:SCW0 + 3 * k + 3]),
                        r=[cx], w=[sacc], c=[pp])
                    for w_ in range(2):
                        S.op("dve", lambda e, k=k, w_=w_: e.scalar_tensor_tensor(
                            out=sacc[:, k, 0:T], in0=cx[:, k, w_:w_ + T], scalar=pp[:, SCW0 + 3 * k + w_:SCW0 + 3 * k + w_ + 1],
                            in1=sacc[:, k, 0:T], op0=ALU.mult, op1=ALU.add), r=[cx, sacc], w=[sacc], c=[pp])
                if not meta:
                    S.op("pool", lambda e: e.tensor_tensor(out=YT[:, 12:16, 0:T], in0=sacc[:, :, 0:T], in1=scb[:, :, 0:T], op=ALU.mult),
                         r=[sacc, scb], w=[YT])
                yield
                if meta:
                    S.op("pool", lambda e: e.tensor_copy(out=halo_x[:, :, :], in_=xpre[:, :, T:T + 3]), r=[xpre], w=[halo_x])
                    S.op("pool", lambda e: e.tensor_copy(out=halo_c[:, :, :], in_=cx[:, :, T:T + 2]), r=[cx], w=[halo_c])
            def backA(seq, ti, T, meta, par, pump, sj=0):
                first = (ti == 0) and not meta
                last = (ti == ntile - 1) and not meta
                YT = YTs[par]
                xact = xacts[par]
                sz = szs[sj]
                dtf = dtfs[sj]
                pool = bpool
                for hb in range(2):
                    pt = pool.get()
                    ptb = pt.t[:].bitcast(BF16)
                    for j in range(8):
                        cc = hb * 8 + j
                        S.op("pe", lambda e, j=j, cc=cc, ptb=ptb, pt=pt: e.transpose(
                            out=ptb[0:T, j * 128:(j + 1) * 128], in_=xact[:, cc, 0:T], identity=identb[:, :]),
                            r=[xact], w=[pt], c=[identb])
                    if hb == 0:
                        S.op("act", lambda e, ptb=ptb, pt=pt: e.activation(out=xsB[0:T, 0:1024], in_=ptb[0:T, :], func=AF.Copy), r=[pt], w=[xsB])
                    else:
                        S.op("dve", lambda e, ptb=ptb, pt=pt: e.tensor_copy(out=xsB[0:T, 1024:2048], in_=ptb[0:T, :]), r=[pt], w=[xsB])
                pump(2)
                p = pool.get()
                S.op("pe", lambda e, p=p: e.matmul(p[0:T, 0:NH], lhsT=cst[0:T, TRIU0:TRIU0 + T], rhs=dtf[0:T, 1, :], start=True, stop=True),
                     r=[dtf], w=[p], c=[cst])
                S.op("pe", lambda e, p=p: e.matmul(p[0:T, 32:32 + NH], lhsT=cst[0:T, ONES0:ONES0 + T], rhs=dtf[0:T, 1, :], start=True, stop=True),
                     r=[dtf], w=[p], c=[cst])
                S.op("dve", lambda e, p=p: e.tensor_copy(out=dts[0:T, 2, :], in_=p[0:T, 0:NH]), r=[p], w=[dts])
                S.op("act", lambda e, p=p: e.activation(out=dts[0:T, 3, :], in_=p[0:T, 0:NH], func=AF.Exp), r=[p], w=[dts])
                S.op("act", lambda e, p=p: e.activation(out=dts[0:T, 5, :], in_=p[0:T, 32:32 + NH], func=AF.Exp), r=[p], w=[dts])
                S.op("dve", lambda e, p=p: e.tensor_tensor(out=dts[0:T, 6, :], in0=p[0:T, 32:32 + NH], in1=dts[0:T, 2, :], op=ALU.subtract),
                     r=[p, dts], w=[dts])
                S.op("act", lambda e: e.activation(out=dts[0:T, 4, :], in_=dts[0:T, 6, :], func=AF.Exp), r=[dts], w=[dts])

                pump(2)

                def hb3(ap2, nh=NH):
                    return ap2.rearrange("p (h q) -> p h q", h=nh)

                def bch(ap2, nh=NH):
                    return ap2.unsqueeze(2).to_broadcast([T, nh, HP])

                S.op("dve", lambda e: e.tensor_tensor(out=hb3(xdt[0:T, :]), in0=hb3(xsB[0:T, 0:DSSD]), in1=bch(dtf[0:T, 0, :]), op=ALU.mult),
                     r=[xsB, dtf], w=[xdt])
                S.op("pool", lambda e: e.tensor_tensor(out=hb3(xdd[0:T, :]), in0=hb3(xdt[0:T, :]), in1=bch(dts[0:T, 4, :]), op=ALU.mult),
                     r=[xdt, dts], w=[xdd])
                if not meta:
                    for g in range(NG):
                        p = pool.get()
                        S.op("pe", lambda e, p=p, g=g: e.matmul(p[0:T, 0:384], lhsT=xact[:, 16 + g, 0:T], rhs=Sbf[:, g * 384:(g + 1) * 384],
                                                                start=True, stop=True), r=[xact, Sbf], w=[p])
                        S.op("dve", lambda e, p=p, g=g: e.tensor_tensor(
                            out=hb3(t1[0:T, g * 384:(g + 1) * 384], 6), in0=hb3(p[0:T, 0:384], 6),
                            in1=bch(dts[0:T, 3, 6 * g:6 * g + 6], 6), op=ALU.mult), r=[p, dts], w=[t1])
                        pump(2)
                    S.op("pool", lambda e: e.tensor_tensor(out=hb3(t1b[0:T, :]), in0=hb3(xsB[0:T, 0:DSSD]),
                                                           in1=bch(bpa[0:T, DSK0:DSK0 + NH]), op=ALU.mult), r=[xsB], w=[t1b], c=[bpa])
                    S.op("dve", lambda e: e.tensor_tensor(out=t1[0:T, :], in0=t1[0:T, :], in1=t1b[0:T, :], op=ALU.add), r=[t1, t1b], w=[t1])
                if not last:
                    dstS = Smeta if meta else St
                    if not meta:
                        S.op("pool", lambda e: e.tensor_tensor(out=hb3(Stmp[:, :]), in0=hb3(St[:, :]),
                                                               in1=dts[:, 5, :].unsqueeze(2).to_broadcast([128, NH, HP]), op=ALU.mult),
                             r=[St, dts], w=[Stmp])
                    for g in range(NG):
                        p = pool.get()
                        S.op("pe", lambda e, p=p, g=g: e.matmul(p[:, 0:384], lhsT=xsB[0:T, DSSD + g * 128:DSSD + (g + 1) * 128],
                                                                rhs=xdd[0:T, g * 384:(g + 1) * 384], start=True, stop=True),
                             r=[xsB, xdd], w=[p])
                        if meta:
                            S.op("dve", lambda e, g=g, p=p: e.tensor_copy(out=dstS[:, g * 384:(g + 1) * 384], in_=p[:, 0:384]),
                                 r=[p], w=[dstS])
                        else:
                            S.op("dve", lambda e, g=g, p=p: e.tensor_tensor(out=St[:, g * 384:(g + 1) * 384], in0=p[:, 0:384],
                                                                            in1=Stmp[:, g * 384:(g + 1) * 384], op=ALU.add),
                                 r=[p, Stmp], w=[St])
                        pump(2)
                    if not meta:
                        S.op("act", lambda e: e.activation(out=Sbf[:, :], in_=St[:, :], func=AF.Copy), r=[St], w=[Sbf])
                if not meta:
                    p = pool.get()
                    for g in range(NG):
                        S.op("pe", lambda e, p=p, g=g: e.matmul(p[0:T, g * 128:g * 128 + T], lhsT=xact[:, 12 + g, 0:T], rhs=xact[:, 16 + g, 0:T],
                                                                start=True, stop=True), r=[xact], w=[p])
                    S.op("act", lambda e, p=p: e.activation(out=cbT[0:T, :], in_=p[0:T, :], func=AF.Copy), r=[p], w=[cbT])
                    pump(2)
                    ybanks = ps[5:8]
                    dbank = {}

                    def unit_head(u):
                        ra = rhsA[u % 2]
                        for j in range(3):
                            h = 3 * u + j
                            S.op("act", lambda e, j=j, h=h: e.activation(
                                out=ra[0:T, j * T:(j + 1) * T], in_=cst[0:T, TRIU0:TRIU0 + T], func=AF.Copy,
                                scale=dtf[0:T, 1, h:h + 1]), r=[dtf], w=[ra], c=[cst])
                        p = pool.get()
                        dbank[u] = p
                        S.op("pe", lambda e: e.matmul(p[0:T, 0:3 * T], lhsT=cst[0:T, ONES0:ONES0 + T], rhs=ra[0:T, 0:3 * T],
                                                      start=True, stop=True), r=[ra], w=[p], c=[cst])

                    unit_head(0)
                    pump(2)
                    for u in range(8):
                        g = u // 2
                        dm = Dm[u % 2]
                        mt = MT[u % 3]
                        if u + 1 < 8:
                            unit_head(u + 1)
                        p = dbank[u]
                        for j in range(3):
                            h = 3 * u + j
                            S.op("dve", lambda e, p=p, dm=dm, j=j, h=h: e.scalar_tensor_tensor(
                                out=dm[0:T, j * T:(j + 1) * T], in0=p[0:T, j * T:(j + 1) * T], scalar=dts[0:T, 2, h:h + 1],
                                in1=cst[0:T, NEGM0:NEGM0 + T], op0=ALU.subtract, op1=ALU.add), r=[p, dts], w=[dm], c=[cst])
                        pump(2)
                        S.op("act", lambda e, dm=dm: e.activation(out=dm[0:T, 0:3 * T], in_=dm[0:T, 0:3 * T], func=AF.Exp), r=[dm], w=[dm])
                        pump(2)
                        S.op("dve", lambda e, dm=dm, mt=mt, g=g: e.tensor_tensor(
                            out=mt[0:T, 0:3 * T].rearrange("p (j l) -> p j l", j=3),
                            in0=dm[0:T, 0:3 * T].rearrange("p (j l) -> p j l", j=3),
                            in1=cbT[0:T, g * 128:g * 128 + T].unsqueeze(1).to_broadcast([T, 3, T]), op=ALU.mult),
                            r=[dm, cbT], w=[mt])
                        for j in range(3):
                            h = 3 * u + j
                            yp = ybanks[h // 8]
                            S.op("pe", lambda e, yp=yp, mt=mt, j=j, h=h: e.matmul(
                                yp[0:T, (h % 8) * 64:(h % 8 + 1) * 64], lhsT=mt[0:T, j * T:(j + 1) * T],
                                rhs=xdt[0:T, h * 64:(h + 1) * 64], start=True, stop=True), r=[mt, xdt], w=[yp])
                        pump(2)
                    pump.mid()
                    for bq in range(3):
                        S.op("dve", lambda e, bq=bq: e.tensor_tensor(out=yb[0:T, bq * 512:(bq + 1) * 512], in0=ybanks[bq][0:T, :],
                                                                     in1=t1[0:T, bq * 512:(bq + 1) * 512], op=ALU.add),
                             r=[ybanks[bq], t1], w=[yb])
                        pump(2)
                    S.op("dve", lambda e: e.tensor_tensor(out=yb[0:T, :], in0=yb[0:T, :], in1=sz[0:T, :], op=ALU.mult), r=[yb, sz], w=[yb])
                    for g in range(NG):
                        S.op("act", lambda e, g=g: e.activation(out=junk2[0:T, 0:384], in_=yb[0:T, g * 384:(g + 1) * 384], func=AF.Square,
                                                                accum_out=smk[0:T, 4 + g:5 + g]), r=[yb], w=[smk, junk2])
                    S.op("act", lambda e: e.activation(out=smk[0:T, 8:12], in_=smk[0:T, 4:8], func=AF.Ln, scale=1.0 / 384, bias=EPS), r=[smk], w=[smk])
                    S.op("act", lambda e: e.activation(out=smk[0:T, 12:16], in_=smk[0:T, 8:12], func=AF.Exp, scale=-0.5), r=[smk], w=[smk])
                    S.op("dve", lambda e: e.tensor_tensor(
                        out=yn[0:T, :].rearrange("p (g q) -> p g q", g=NG), in0=yb[0:T, :].rearrange("p (g q) -> p g q", g=NG),
                        in1=smk[0:T, 12:16].unsqueeze(2).to_broadcast([T, NG, 384]), op=ALU.mult), r=[yb, smk], w=[yn])
                    pump(2)
                    for hb in range(2):
                        nchk = 8 if hb == 0 else 4
                        pt = pool.get()
                        ptb = pt.t[:].bitcast(BF16)
                        for j in range(nchk):
                            cc = hb * 8 + j
                            S.op("pe", lambda e, j=j, cc=cc, ptb=ptb: e.transpose(
                                out=ptb[:, j * 128:j * 128 + T], in_=yn[0:T, cc * 128:(cc + 1) * 128], identity=identb[0:T, 0:T]),
                                r=[yn], w=[pt], c=[identb])
                        S.op("act", lambda e, ptb=ptb, hb=hb, nchk=nchk: e.activation(
                            out=YT[:, hb * 8:hb * 8 + nchk, 0:T], in_=ptb.rearrange("p (k t) -> p k t", k=8)[:, 0:nchk, 0:T], func=AF.Copy),
                            r=[pt], w=[YT])
                        pump(2)
                    S.dma("pool", lambda e: e.dma_start(out=yt_d[seq * NT + ti, :, :], in_=YT[:, :, :].rearrange("p k t -> p (k t)")),
                          dst=yt_b, r=[YT])
                pump(10 ** 6)

            def dump(nm, tl, ap):
                bb = Buf("dbg_" + nm)
                dbg_bufs.append(bb)
                S.dma("pool", lambda e: e.dma_start(out=dbg_d[nm].ap(), in_=ap), dst=bb, r=[tl])

            class FrontRun:
                def __init__(self, gen):
                    self.g = gen
                    self.done = gen is None
                    self.at_mid = False

                def step(self, n, stop_mid=False):
                    for _ in range(n):
                        if self.done or (stop_mid and self.at_mid):
                            return
                        try:
                            v = next(self.g)
                            if v == "mid":
                                self.at_mid = True
                        except StopIteration:
                            self.done = True

                def flush(self):
                    while not self.done:
                        self.step(1000)

            class Pumper:
                def __init__(self, fa, fb):
                    self.fa, self.fb = fa, fb
                    self.acc_a = 0.0
                    self.acc_b = 0.0

                def __call__(self, n=1):
                    if n >= 10 ** 6:
                        if self.fa is not None:
                            self.fa.flush()
                        if self.fb is not None:
                            self.fb.step(10 ** 6, stop_mid=True)
                        return
                    self.acc_a += 0.16 * n
                    self.acc_b += 0.70 * n
                    ka, kb = int(self.acc_a), int(self.acc_b)
                    self.acc_a -= ka
                    self.acc_b -= kb
                    if self.fa is not None and ka:
                        self.fa.step(ka)
                    if self.fb is not None and kb:
                        self.fb.step(kb, stop_mid=True)

                def mid(self):
                    pass

            nopump = Pumper(None, None)
            for _ in frontA(0, 0, NMETA, True, 0):
                pass
            backA(0, 0, NMETA, True, 0, nopump)
            if dbgA:
                dump("Smeta", Smeta, Smeta[:, :])
            tiles = [(sq, ti) for sq in range(nseq) for ti in range(ntile)]
            runs = {}

            def get_run(j):
                if j >= len(tiles):
                    return None
                if j not in runs:
                    runs[j] = FrontRun(frontA(tiles[j][0], tiles[j][1], 128, False, (j + 1) % 2, sj=j % 3, jg=j))
                return runs[j]

            get_run(0).flush()
            if get_run(1) is not None:
                get_run(1).step(10 ** 6, stop_mid=True)
            for i, (sq, ti) in enumerate(tiles):
                par = (i + 1) % 2
                if ti == 0:
                    S.op("pool", lambda e: e.tensor_copy(out=St[:, :], in_=Smeta[:, :]), r=[Smeta], w=[St])
                    S.op("act", lambda e: e.activation(out=Sbf[:, :], in_=Smeta[:, :], func=AF.Copy), r=[Smeta], w=[Sbf])
                uv_issue(1)
                pump = Pumper(get_run(i + 1), get_run(i + 2))
                backA(sq, ti, 128, False, par, pump, sj=i % 3)
                if dbgA and sq == 0 and ti == 0:
                    dump("dts", dts, dts[:, :, :].rearrange("p a h -> p (a h)"))
                    dump("xsB", xsB, xsB[:, :]); dump("xdt", xdt, xdt[:, :]); dump("cbT", cbT, cbT[:, :])
                    dump("t1", t1, t1[:, :]); dump("yb", yb, yb[:, :]); dump("St", St, St[:, :]); dump("sz", szs[i % 3], szs[i % 3][:, :])
                    dump("Dm1", Dm[1], Dm[1][:, :]); dump("MT1", MT[1], MT[1][:, :])
                    dump("xact", xacts[par], xacts[par][:, :, :].rearrange("p k t -> p (k t)"))
            uv_issue(10 ** 6)
            S.barrier()

        if dbgA:
            S.final_wait("sp", [yt_b] + dbg_bufs)
            return nc
        with ExitStack() as eb:
            gpool = Pool(ps[0:4])
            ffnbs = [[ps[4], ps[5]], [ps[6], ps[7]]]
            wout = sb(eb, "wout", [128, 16, D], BF16)
            wq = sb(eb, "wq", [128, 8, 2048], BF16)
            skt = sb(eb, "skt", [128, 16, 128], BF16)
            bpb = sb(eb, "bpb", [128, 2048], F32)
            ppb = sb(eb, "ppb", [128, 136], F32)
            NW0 = 120
            S.dma("sp", lambda e: e.dma_start(out=bpb[:, :], in_=bpb_d.ap().partition_broadcast(128)), dst=bpb)
            S.dma("sp", lambda e: e.dma_start(out=ppb[:, :], in_=pp_d.ap()), dst=ppb)
            with ExitStack() as est:
                stg = [sb(est, "stgb%d" % i, [128, 2048], F32) for i in range(2)]
                n = 0
                for c in range(16):
                    st = stg[n % 2]
                    S.dma("sp", lambda e, st=st, c=c: e.dma_start(out=st[:, 0:D], in_=wout_d[c * 128:(c + 1) * 128, :]), dst=st)
                    eng = "act" if n % 2 == 0 else "dve"
                    if eng == "act":
                        S.op("act", lambda e, st=st, c=c: e.activation(out=wout[:, c, :], in_=st[:, 0:D], func=AF.Copy,
                                                                       scale=ppb[:, NW0 + c:NW0 + c + 1]), r=[st], w=[wout], c=[ppb])
                    else:
                        S.op("dve", lambda e, st=st, c=c: e.tensor_scalar(out=wout[:, c, :], in0=st[:, 0:D], scalar1=ppb[:, NW0 + c:NW0 + c + 1],
                                                                          scalar2=None, op0=ALU.mult), r=[st], w=[wout], c=[ppb])
                    n += 1
                for k in range(8):
                    st = stg[n % 2]
                    S.dma("sp", lambda e, st=st, k=k: e.dma_start(out=st[:, :], in_=wq_d[k * 128:(k + 1) * 128, :]), dst=st)
                    if n % 2 == 0:
                        S.op("act", lambda e, st=st, k=k: e.activation(out=wq[:, k, :], in_=st[:, :], func=AF.Copy), r=[st], w=[wq])
                    else:
                        S.op("dve", lambda e, st=st, k=k: e.tensor_copy(out=wq[:, k, :], in_=st[:, :]), r=[st], w=[wq])
                    n += 1
                st = stg[n % 2]
                S.dma("sp", lambda e, st=st: e.dma_start(out=st[:, :], in_=skt_d.ap()), dst=st)
                S.op("dve", lambda e, st=st: e.tensor_copy(out=skt[:, :, :].rearrange("p j k -> p (j k)"), in_=st[:, :]), r=[st], w=[skt])
                S.barrier()
            GF0, GL0 = 0, 1024
            ytl = [sb(eb, "ytl%d" % i, [128, 16, 128], BF16) for i in range(2)]
            xtb = [sb(eb, "xtb%d" % i, [128, D], F32) for i in range(2)]
            h1s = [sb(eb, "h1_%d" % i, [128, D], F32) for i in range(2)]
            xn2s = [sb(eb, "xn2_%d" % i, [128, D], BF16) for i in range(2)]
            idss = [sb(eb, "ids%d" % i, [128, NSLOT], I32) for i in range(2)]
            gatess = [sb(eb, "gates%d" % i, [128, NSLOT], F32) for i in range(2)]
            junkb = sb(eb, "junkb", [128, D], BF16)
            smb = sb(eb, "smb", [128, 16], F32)
            smo = sb(eb, "smo", [128, 4], F32)
            mhalf = sb(eb, "mhalf", [128, 1], F32)
            S.op("pool", lambda e: e.memset(mhalf[:, :], -0.5), w=[mhalf])
            n2T = sb(eb, "n2T", [128, 8, 128], BF16)
            qT = sb(eb, "qT", [128, 16, 128], BF16)
            ssb = sb(eb, "ssb", [128, 16, 128], F32)
            ss2 = sb(eb, "ss2", [128, 16, 128], F32)
            vv = sb(eb, "vv", [128, 16, 16], F32)
            ixu = sb(eb, "ixu", [128, 16, 16], U32)
            ixf = sb(eb, "ixf", [128, 16, 16], F32)
            cand = Tl(ssb.t[:, :, :].rearrange("p (h two) k -> p h (two k)", two=2), "cand_alias")
            cand.b = ssb.b
            cand2 = Tl(ss2.t[:, :, :].rearrange("p (h two) k -> p h (two k)", two=2), "cand2_alias")
            cand2.b = ss2.b
            best = sb(eb, "best", [128, 8, 16], F32)
            posu = sb(eb, "posu", [128, 8, 16], U32)
            pa = sb(eb, "pa", [128, 128], U32)
            pb = sb(eb, "pb", [128, 128], U32)
            paf = sb(eb, "paf", [128, 128], F32)
            pbf = sb(eb, "pbf", [128, 128], F32)
            oh = sb(eb, "oh", [128, 128, 16], F32)
            i1 = sb(eb, "i1", [128, 128], F32)
            i2 = sb(eb, "i2", [128, 128], F32)
            ex = sb(eb, "ex", [128, 8, 16], F32)
            hidr = [sb(eb, "hid%d" % i, [128, 4], F32) for i in range(6)]
            UVb = [sb(eb, "UVb%d" % i, [128, 2 * D], BF16) for i in range(NGB)]
            dg = [sb(eb, "dg%d" % i, [128, 128], BF16) for i in range(6)]
            ot = [sb(eb, "ot%d" % i, [128, D], F32) for i in range(2)]
            junkfs = [sb(eb, "junkf%d" % i, [128, D], BF16) for i in range(3)]

            vvB = [Buf("vv%d" % j) for j in range(16)]
            ixB = [Buf("ix%d" % j) for j in range(16)]
            s2B = [Buf("s2_%d" % j) for j in range(16)]
            beB = [Buf("be%d" % h) for h in range(8)]
            poB = [Buf("po%d" % h) for h in range(8)]
            c2B = [Buf("c2_%d" % h) for h in range(8)]

            def stageA(it):
                seq, ti = divmod(it, ntile)
                par = it % 2
                yl, xt, h1, xn2, ids, gates = ytl[par], xtb[par], h1s[par], xn2s[par], idss[par], gatess[par]
                S.dma("sp", lambda e: e.dma_start(out=yl[:, :, :].rearrange("p k t -> p (k t)"), in_=yt_d[seq * NT + ti, :, :]),
                      dst=yl, r=[yt_b])
                S.dma("sp", lambda e: e.dma_start(out=xt[:, :], in_=x_d[seq, ti * 128:(ti + 1) * 128, :]), dst=xt)
                yield
                for nb in range(2):
                    p = gpool.get()
                    for c in range(16):
                        S.op("pe", lambda e, p=p, c=c, nb=nb: e.matmul(p[:, :], lhsT=yl[:, c, :], rhs=wout[:, c, nb * 512:(nb + 1) * 512],
                                                                       start=(c == 0), stop=(c == 15)), r=[yl], w=[p], c=[wout])
                        if c % 2 == 1:
                            yield
                    S.op("dve", lambda e, p=p, nb=nb: e.tensor_tensor(out=h1[:, nb * 512:(nb + 1) * 512], in0=p[:, :],
                                                                      in1=xt[:, nb * 512:(nb + 1) * 512], op=ALU.add), r=[p, xt], w=[h1])
                    yield
                S.op("act", lambda e: e.activation(out=junkb[:, :], in_=h1[:, :], func=AF.Square, accum_out=smb[:, 0:1]), r=[h1], w=[smb, junkb])
                S.op("pool", lambda e: e.tensor_scalar(out=smb[:, 1:2], in0=smb[:, 0:1], scalar1=1.0 / D, scalar2=EPS, op0=ALU.mult, op1=ALU.add),
                     r=[smb], w=[smb])
                S.op("pool", lambda e: e.tensor_tensor(out=smb[:, 2:3], in0=smb[:, 1:2], in1=mhalf[:, 0:1], op=ALU.pow), r=[smb], w=[smb], c=[mhalf])
                yield
                S.op("dve", lambda e: e.scalar_tensor_tensor(out=xn2[:, :], in0=h1[:, :], scalar=smb[:, 2:3], in1=bpb[:, GF0:GF0 + D],
                                                             op0=ALU.mult, op1=ALU.mult), r=[h1, smb], w=[xn2], c=[bpb])
                yield
                pt = gpool.get()
                ptb = pt.t[:].bitcast(BF16)
                for k in range(8):
                    S.op("pe", lambda e, k=k: e.transpose(out=ptb[:, k * 128:(k + 1) * 128], in_=xn2[:, k * 128:(k + 1) * 128],
                                                          identity=identb[:, :]), r=[xn2], w=[pt], c=[identb])
                yield
                S.op("act", lambda e: e.activation(out=n2T[:, :, :].rearrange("p k t -> p (k t)"), in_=ptb[:, :], func=AF.Copy), r=[pt], w=[n2T])
                yield
                for qb in range(4):
                    p = gpool.get()
                    for j in range(4):
                        jj = qb * 4 + j
                        for k in range(8):
                            S.op("pe", lambda e, p=p, j=j, jj=jj, k=k: e.matmul(
                                p[:, j * 128:(j + 1) * 128], lhsT=wq[:, k, jj * 128:(jj + 1) * 128], rhs=n2T[:, k, :],
                                start=(k == 0), stop=(k == 7)), r=[n2T], w=[p], c=[wq])
                        yield
                    S.op("act", lambda e, p=p, qb=qb: e.activation(out=qT[:, 4 * qb:4 * qb + 4, :].rearrange("p j t -> p (j t)"), in_=p[:, :],
                                                                   func=AF.Copy), r=[p], w=[qT])
                    yield
                for qb in range(4):
                    p = gpool.get()
                    for j in range(4):
                        jj = qb * 4 + j
                        S.op("pe", lambda e, p=p, j=j, jj=jj: e.matmul(p[:, j * 128:(j + 1) * 128], lhsT=qT[:, jj, :], rhs=skt[:, jj, :],
                                                                       start=True, stop=True), r=[qT], w=[p], c=[skt])
                    yield
                    S.op("act", lambda e, p=p, qb=qb: e.activation(out=ssb[:, 4 * qb:4 * qb + 4, :].rearrange("p j t -> p (j t)"), in_=p[:, :],
                                                                   func=AF.Copy), r=[p], w=[ssb])
                    yield
                def topk_steps(vals, vals2, vB, iB, v2B, outv, outi, idxs, alias_w=lambda j: []):
                    steps = []
                    for j in idxs:
                        steps.append(lambda j=j: S.op("dve", lambda e: e.max(out=outv[:, j, 0:8], in_=vals[:, j, :]), r=[vals], w=[vB[j]]))
                    for j in idxs:
                        steps.append(lambda j=j: S.op("dve", lambda e: e.max_index(out=outi[:, j, 0:8], in_max=outv[:, j, 0:8], in_values=vals[:, j, :]),
                                                      r=[vals, vB[j]], w=[iB[j]]))
                    for j in idxs:
                        steps.append(lambda j=j: S.op("dve", lambda e: e.match_replace(out=vals2[:, j, :], in_to_replace=outv[:, j, 0:8],
                                                                                        in_values=vals[:, j, :], imm_value=-1e30),
                                                      r=[vals, vB[j]], w=[v2B[j]] + alias_w(j)))
                    for j in idxs:
                        steps.append(lambda j=j: S.op("dve", lambda e: e.max(out=outv[:, j, 8:16], in_=vals2[:, j, :]), r=[v2B[j]], w=[vB[j]]))
                    for j in idxs:
                        steps.append(lambda j=j: S.op("dve", lambda e: e.max_index(out=outi[:, j, 8:16], in_max=outv[:, j, 8:16], in_values=vals2[:, j, :]),
                                                      r=[v2B[j], vB[j]], w=[iB[j]]))
                    return steps

                for j0 in range(0, 16, 4):
                    st = topk_steps(ssb, ss2, vvB, ixB, s2B, vv, ixu, range(j0, j0 + 4), alias_w=lambda j: [c2B[j // 2]])
                    for i_, f_ in enumerate(st):
                        f_()
                        if i_ % 3 == 2:
                            yield
                    yield
                S.op("dve", lambda e: e.tensor_copy(out=ixf[:, :, :], in_=ixu[:, :, :]), r=ixB, w=[ixf])
                v4 = vv[:, :, :].rearrange("p (h two) k -> p h two k", two=2)
                S.op("dve", lambda e: e.tensor_tensor(
                    out=cand[:, :, :].rearrange("p h (a b) -> p h a b", a=16),
                    in0=v4[:, :, 0, :].unsqueeze(3).to_broadcast([128, 8, 16, 16]),
                    in1=v4[:, :, 1, :].unsqueeze(2).to_broadcast([128, 8, 16, 16]), op=ALU.add), r=vvB, w=[cand])
                yield
                for h0 in range(0, 8, 4):
                    st = topk_steps(cand, cand2, beB, poB, c2B, best, posu, range(h0, h0 + 4), alias_w=lambda h: [s2B[2 * h], s2B[2 * h + 1]])
                    for i_, f_ in enumerate(st):
                        f_()
                        if i_ % 3 == 2:
                            yield
                    yield
                S.op("dve", lambda e: e.tensor_tensor(out=ex[:, :, :], in0=best[:, :, :], in1=best[:, :, 0:1].to_broadcast([128, 8, 16]),
                                                      op=ALU.subtract), r=beB, w=[ex])
                yield
                S.op("act", lambda e: e.activation(out=ex[:, :, :], in_=ex[:, :, :], func=AF.Exp), r=[ex], w=[ex])
                yield
                S.op("dve", lambda e: e.tensor_reduce(out=smb[:, 4:12], in_=ex[:, :, :], axis=AX.X, op=ALU.add), r=[ex], w=[smb])
                S.op("dve", lambda e: e.reciprocal(out=smb[:, 4:12], in_=smb[:, 4:12]), r=[smb], w=[smb])
                S.op("dve", lambda e: e.tensor_tensor(out=gates[:, :].rearrange("p (h k) -> p h k", h=8), in0=ex[:, :, :],
                                                      in1=smb[:, 4:12].unsqueeze(2).to_broadcast([128, 8, 16]), op=ALU.mult),
                     r=[ex, smb], w=[gates])
                yield
                pos2 = posu[:, :, :].rearrange("p h k -> p (h k)")
                S.op("dve", lambda e: e.tensor_single_scalar(out=pa[:, :], in_=pos2, scalar=4, op=ALU.logical_shift_right), r=poB, w=[pa])
                S.op("dve", lambda e: e.tensor_single_scalar(out=pb[:, :], in_=pos2, scalar=15, op=ALU.bitwise_and), r=poB, w=[pb])
                S.op("dve", lambda e: e.tensor_copy(out=paf[:, :], in_=pa[:, :]), r=[pa], w=[paf])
                S.op("dve", lambda e: e.tensor_copy(out=pbf[:, :], in_=pb[:, :]), r=[pb], w=[pbf])
                yield
                ix4 = ixf[:, :, :].rearrange("p (h two) k -> p h two k", two=2)
                for half, (pf, idst) in enumerate([(paf, i1), (pbf, i2)]):
                    S.op("dve", lambda e, pf=pf: e.tensor_tensor(
                        out=oh[:, :, :], in0=cst[:, IOTA0:IOTA0 + 16].unsqueeze(1).to_broadcast([128, 128, 16]),
                        in1=pf[:, :].unsqueeze(2).to_broadcast([128, 128, 16]), op=ALU.is_equal), r=[pf], w=[oh], c=[cst])
                    yield
                    S.op("dve", lambda e, half=half: e.tensor_tensor(
                        out=oh[:, :, :].rearrange("p (h k) a -> p h k a", h=8), in0=oh[:, :, :].rearrange("p (h k) a -> p h k a", h=8),
                        in1=ix4[:, :, half, :].unsqueeze(2).to_broadcast([128, 8, 16, 16]), op=ALU.mult), r=[oh, ixf], w=[oh])
                    yield
                    S.op("dve", lambda e, idst=idst: e.tensor_reduce(out=idst[:, :], in_=oh[:, :, :], axis=AX.X, op=ALU.add), r=[oh], w=[idst])
                    yield
                S.op("dve", lambda e: e.scalar_tensor_tensor(out=i1[:, :], in0=i1[:, :], scalar=128.0, in1=i2[:, :], op0=ALU.mult, op1=ALU.add),
                     r=[i1, i2], w=[i1])
                S.op("dve", lambda e: e.tensor_copy(out=ids[:, :], in_=i1[:, :]), r=[i1], w=[ids])
                yield

            def stageB(it, agen):
                seq, ti = divmod(it, ntile)
                par = it % 2
                h1, xn2, ids, gates = h1s[par], xn2s[par], idss[par], gatess[par]
                o = ot[par]
                h2 = o
                ffnb = ffnbs[par]

                def pump(n=1):
                    if agen is not None:
                        for _ in range(n):
                            try:
                                next(agen)
                            except StopIteration:
                                return

                for k in range(NSLOT):
                    uvb = UVb[k % NGB]
                    hk = hidr[k % 6]
                    d = dg[k % 6]
                    S.dma("pool", lambda e, uvb=uvb, k=k: e.indirect_dma_start(
                        out=uvb[:, :], out_offset=None, in_=uv_d[:, :],
                        in_offset=bass.IndirectOffsetOnAxis(ap=ids[:, k:k + 1], axis=0)), dst=uvb, r=[ids], c=uv_bufs)
                    jf = junkfs[k % 3]
                    S.op("dve", lambda e, uvb=uvb, hk=hk, jf=jf: e.scalar_tensor_tensor(
                        out=jf[:, :], in0=uvb[:, 0:D], scalar=1.0, in1=xn2[:, :], op0=ALU.mult, op1=ALU.mult,
                        accum_out=hk[:, 0:1]), r=[uvb, xn2], w=[hk, jf])
                    S.op("act", lambda e, hk=hk: e.activation(out=hk[:, 1:2], in_=hk[:, 0:1], func=AF.Gelu), r=[hk], w=[hk])
                    S.op("act", lambda e, hk=hk, k=k: e.activation(out=hk[:, 2:3], in_=hk[:, 1:2], func=AF.Copy, scale=gates[:, k:k + 1]),
                         r=[hk, gates], w=[hk])
                    S.op("act", lambda e, d=d, hk=hk: e.activation(out=d[:, :], in_=cst[:, 0:128], func=AF.Copy, scale=hk[:, 2:3]),
                         r=[hk], w=[d], c=[cst])
                    for nb in range(2):
                        S.op("pe", lambda e, d=d, uvb=uvb, nb=nb, k=k: e.matmul(
                            ffnb[nb][:, :], lhsT=d[:, :], rhs=uvb[:, D + nb * 512:D + (nb + 1) * 512], start=(k == 0), stop=(k == NSLOT - 1)),
                            r=[d, uvb], w=[ffnb[nb]])
                    pump(1)
                pump(100000)
                for nb in range(2):
                    S.op("dve", lambda e, nb=nb: e.tensor_tensor(out=h2[:, nb * 512:(nb + 1) * 512], in0=ffnb[nb][:, :],
                                                                 in1=h1[:, nb * 512:(nb + 1) * 512], op=ALU.add), r=[ffnb[nb], h1], w=[h2])
                S.op("act", lambda e: e.activation(out=junkb[:, :], in_=h2[:, :], func=AF.Square, accum_out=smo[:, 0:1]), r=[h2], w=[smo, junkb])
                S.op("pool", lambda e: e.tensor_scalar(out=smo[:, 1:2], in0=smo[:, 0:1], scalar1=1.0 / D, scalar2=EPS, op0=ALU.mult, op1=ALU.add),
                     r=[smo], w=[smo])
                S.op("pool", lambda e: e.tensor_tensor(out=smo[:, 2:3], in0=smo[:, 1:2], in1=mhalf[:, 0:1], op=ALU.pow), r=[smo], w=[smo], c=[mhalf])
                S.op("dve", lambda e: e.scalar_tensor_tensor(out=o[:, :], in0=h2[:, :], scalar=smo[:, 2:3], in1=bpb[:, GL0:GL0 + D],
                                                             op0=ALU.mult, op1=ALU.mult), r=[h2, smo], w=[o], c=[bpb])
                S.dma("sp", lambda e: e.dma_start(out=out_d[seq, ti * 128:(ti + 1) * 128, :], in_=o[:, :]), dst=out_b, r=[o])

            ntot = nseq * ntile
            g0 = stageA(0)
            for _ in g0:
                pass
            for it in range(ntot):
                agen = stageA(it + 1) if it + 1 < ntot else None
                stageB(it, agen)
            S.final_wait("sp", [out_b])
    return nc


def _host_inputs(inputs):
    f = lambda a: np.ascontiguousarray(np.asarray(a, dtype=np.float32))
    x = f(inputs["x"])
    pp = np.concatenate([
        f(inputs["g_mix"])[0].reshape(8, 128).T,
        f(inputs["conv_ssd_w"])[0].T.reshape(20, 128, 4).transpose(1, 0, 2).reshape(128, 80),
        f(inputs["conv_ssd_b"])[0].reshape(20, 128).T,
        f(inputs["conv_sc_w"])[0].T.reshape(4, 128, 3).transpose(1, 0, 2).reshape(128, 12),
        np.concatenate([f(inputs["ssd_norm_w"])[0], np.ones(512, np.float32)]).reshape(16, 128).T,
    ], axis=1)
    bpa = np.concatenate([f(inputs["dt_bias"])[0], f(inputs["a_log"])[0], f(inputs["d_skip"])[0]])[None, :]
    bpb = np.concatenate([f(inputs["g_ffn"])[0], f(inputs["g_final"])])[None, :]
    skt = f(inputs["sub_keys"])[0].transpose(3, 0, 1, 2).reshape(128, 2048)
    r = np.arange(128)
    cst = np.concatenate([
        np.eye(128, dtype=np.float32),
        (r[:, None] <= r[None, :]).astype(np.float32),
        np.ones((128, 128), np.float32),
        np.where(r[None, :] < r[:, None], np.float32(NEG), np.float32(0.0)).astype(np.float32),
        np.broadcast_to(np.arange(16, dtype=np.float32)[None, :], (128, 16)),
    ], axis=1)
    shared = {
        "meta": f(inputs["meta_tokens"]), "w_in": f(inputs["w_in"])[0], "w_out": f(inputs["w_out"])[0],
        "w_q": f(inputs["w_q"])[0], "skt": np.ascontiguousarray(skt), "expert_u": f(inputs["expert_u"])[0],
        "expert_v": f(inputs["expert_v"])[0], "pp": np.ascontiguousarray(pp), "bpa": np.ascontiguousarray(bpa),
        "bpb": np.ascontiguousarray(bpb), "cst": np.ascontiguousarray(cst),
    }
    return x, shared


def kernel(**inputs):
    x, shared = _host_inputs(inputs)
    ncores = 8
    nc = build()
    in_maps = []
    for c in range(ncores):
        m = dict(shared)
        m["x"] = np.ascontiguousarray(x[c * NSEQ:(c + 1) * NSEQ])
        in_maps.append(m)
    res = run_bass_kernel_spmd(nc, in_maps, core_ids=list(range(ncores)))
    out = np.concatenate([np.asarray(r["out"], dtype=np.float32) for r in res.results], axis=0)
    return out
```

```python
import numpy as np
from contextlib import ExitStack
import concourse.bass as bass
import concourse.mybir as mybir
from concourse.bass_utils import run_bass_kernel_spmd

F32 = mybir.dt.float32
BF16 = mybir.dt.bfloat16
I32 = mybir.dt.int32
U32 = mybir.dt.uint32
AF = mybir.ActivationFunctionType
ALU = mybir.AluOpType
AX = mybir.AxisListType

D = 1024
SEQ = 2048
NSEQ = 2
NT = SEQ // 128
NMETA = 16
DIN = 5656
NH = 24
HP = 64
NG = 4
DSSD = 1536
EPS = 1e-6
C_Z, C_XBC, C_DT, C_SCB, C_SCC, C_SCX = 0, 1536, 4096, 4120, 4632, 5144
NSLOT = 128
NEXP = 16384
NGB = 10
NEG = -30000.0
STRICT_SAME_ENGINE = True


class Buf:
    __slots__ = ("name", "w", "r", "dsem", "dcnt")

    def __init__(self, name):
        self.name = name
        self.w = None
        self.r = []
        self.dsem = None
        self.dcnt = 0


class Tl:
    def __init__(self, t, name):
        self.t = t
        self.b = Buf(name)

    def __getitem__(self, k):
        return self.t[k]


def _b(x):
    return x.b if isinstance(x, Tl) else x


class Sched:
    def __init__(self, nc, es):
        self.nc = nc
        self.es = es
        self.eng = {"pe": nc.tensor, "act": nc.scalar, "dve": nc.vector, "pool": nc.gpsimd, "sp": nc.sync}
        self.sem = {k: es.enter_context(nc.semaphore("s_" + k)) for k in self.eng}
        self.cnt = {k: 0 for k in self.eng}
        self.seen = {k: {} for k in self.eng}
        self.dbufs = []
        self.nsem = 0

    def _wait(self, e, deps):
        best = {}
        for (s, v) in deps:
            key = id(s)
            if key not in best or v > best[key][1]:
                best[key] = (s, v)
        for key, (s, v) in best.items():
            if self.seen[e].get(key, 0) >= v:
                continue
            self.eng[e].wait_ge(s, v)
            self.seen[e][key] = v

    def _deps(self, r, w, c, own=None, disjoint=False):
        deps = []
        for b in r:
            if b.w is not None:
                deps.append(b.w)
        for b in c:
            if b.w is not None:
                deps.append(b.w)
        for b in w:
            if b.w is not None and ((STRICT_SAME_ENGINE and not disjoint) or b.w[0] is not own):
                deps.append(b.w)
            deps.extend(x for x in b.r if STRICT_SAME_ENGINE or x[0] is not own)
        return deps

    def op(self, e, fn, r=(), w=(), c=(), disjoint=False):
        r = [_b(x) for x in r]
        w = [_b(x) for x in w]
        c = [_b(x) for x in c]
        deps = self._deps(r, w, c, own=self.sem[e], disjoint=disjoint)
        if e == "pe":
            deps = [d for d in deps if d[0] is not self.sem["pe"]]
        self._wait(e, deps)
        inst = fn(self.eng[e])
        self.cnt[e] += 1
        inst.then_inc(self.sem[e], 1)
        t = (self.sem[e], self.cnt[e])
        for b in w:
            b.w = t
            b.r = []
        for b in r:
            b.r.append(t)
        return inst

    def dma(self, q, fn, dst, r=(), c=()):
        dst = _b(dst)
        r = [_b(x) for x in r]
        c = [_b(x) for x in c]
        deps = self._deps(r, [dst], c)
        self._wait(q, deps)
        if dst.dsem is None:
            dst.dsem = self.es.enter_context(self.nc.semaphore("d%d_%s" % (self.nsem, dst.name)))
            self.nsem += 1
            self.dbufs.append(dst)
        inst = fn(self.eng[q])
        dst.dcnt += 16
        inst.then_inc(dst.dsem, 16)
        t = (dst.dsem, dst.dcnt)
        dst.w = t
        dst.r = []
        for b in r:
            b.r.append(t)
        return inst

    def barrier(self):
        allv = [(self.sem[k], self.cnt[k]) for k in self.eng if self.cnt[k] > 0]
        allv += [(b.dsem, b.dcnt) for b in self.dbufs]
        for e in self.eng:
            self._wait(e, [d for d in allv if d[0] is not self.sem[e]])

    def final_wait(self, e, bufs):
        self._wait(e, [(_b(b).dsem, _b(b).dcnt) for b in bufs])


class Pool:
    def __init__(self, tiles):
        self.tiles = tiles
        self.i = 0

    def get(self):
        t = self.tiles[self.i % len(self.tiles)]
        self.i += 1
        return t


def build(debug=None, nseq=NSEQ, ntile=NT):
    nc = bass.Bass("TRN2", target_bir_lowering=False)
    dbgA = debug == "A"
    x_d = nc.dram_tensor("x", [NSEQ, SEQ, D], F32, kind="ExternalInput")
    meta_d = nc.dram_tensor("meta", [NMETA, D], F32, kind="ExternalInput")
    win_d = nc.dram_tensor("w_in", [D, DIN], F32, kind="ExternalInput")
    wout_d = nc.dram_tensor("w_out", [2048, D], F32, kind="ExternalInput")
    wq_d = nc.dram_tensor("w_q", [D, 2048], F32, kind="ExternalInput")
    skt_d = nc.dram_tensor("skt", [128, 2048], F32, kind="ExternalInput")
    eu_d = nc.dram_tensor("expert_u", [NEXP, D], F32, kind="ExternalInput")
    ev_d = nc.dram_tensor("expert_v", [NEXP, D], F32, kind="ExternalInput")
    pp_d = nc.dram_tensor("pp", [128, 136], F32, kind="ExternalInput")
    bpa_d = nc.dram_tensor("bpa", [1, 72], F32, kind="ExternalInput")
    bpb_d = nc.dram_tensor("bpb", [1, 2048], F32, kind="ExternalInput")
    cst_d = nc.dram_tensor("cst", [128, 528], F32, kind="ExternalInput")
    out_d = nc.dram_tensor("out", [NSEQ, SEQ, D], F32, kind="ExternalOutput")
    yt_d = nc.dram_tensor("yt_scr", [NSEQ * NT, 128, 2048], BF16,
                          **({"kind": "ExternalOutput"} if dbgA else {}))

    uv_d = nc.dram_tensor("uv_scr", [NEXP, 2 * D], BF16)
    dbg_d = {}
    if dbgA:
        for nm, shp, dt_ in [("dts", [128, 192], F32), ("xsB", [128, 2048], BF16), ("xdt", [128, 1536], BF16),
                             ("cbT", [128, 512], F32), ("t1", [128, 1536], F32), ("yb", [128, 1536], F32),
                             ("St", [128, 1536], F32), ("Smeta", [128, 1536], F32), ("sz", [128, 1536], BF16),
                             ("Dm1", [128, 384], F32), ("MT1", [128, 384], BF16), ("xact", [128, 2560], BF16)]:
            dbg_d[nm] = nc.dram_tensor("dbg_" + nm, shp, dt_, kind="ExternalOutput")
    dbg_bufs = []
    with ExitStack() as es:
        S = Sched(nc, es)

        def sb(stack, name, shape, dt):
            return Tl(stack.enter_context(nc.sbuf_tensor("sb_" + name, shape, dt)), name)

        ps = [Tl(es.enter_context(nc.psum_tensor("ps%d" % i, [128, 512], F32)), "ps%d" % i) for i in range(8)]
        yt_b = Buf("yt_scr")
        out_b = Buf("out")

        cst = sb(es, "cst", [128, 528], F32)
        identb = sb(es, "identb", [128, 128], BF16)
        S.dma("sp", lambda e: e.dma_start(out=cst[:, :], in_=cst_d.ap()), dst=cst)
        S.op("dve", lambda e: e.tensor_copy(out=identb[:, :], in_=cst[:, 0:128]), r=[cst], w=[identb])
        ident = cst
        TRIU0, ONES0, NEGM0, IOTA0 = 128, 256, 384, 512
        uv_bufs = [Buf("uv%d" % i) for i in range(4)]
        UVCH = 1024
        uv_jobs = [(r0, half) for r0 in range(0, NEXP, UVCH) for half in range(2)]
        uv_state = {"i": 0}

        def uv_issue(n):
            for _ in range(n):
                i = uv_state["i"]
                if dbgA or i >= len(uv_jobs):
                    return
                r0, half = uv_jobs[i]
                src_d = eu_d if half == 0 else ev_d
                S.dma("pool", lambda e: e.dma_start(out=uv_d[r0:r0 + UVCH, half * D:(half + 1) * D], in_=src_d[r0:r0 + UVCH, :]),
                      dst=uv_bufs[i % 4])
                uv_state["i"] = i + 1

        with ExitStack() as ea:
            fpool = Pool(ps[0:2])
            bpool = Pool(ps[2:5])
            win = sb(ea, "win", [128, 8, DIN], BF16)
            pp = sb(ea, "pp", [128, 136], F32)
            bpa = sb(ea, "bpa", [128, 72], F32)
            A_bc = sb(ea, "A_bc", [128, NH], F32)
            S.dma("sp", lambda e: e.dma_start(out=pp[:, :], in_=pp_d.ap()), dst=pp)
            S.dma("sp", lambda e: e.dma_start(out=bpa[:, :], in_=bpa_d.ap().partition_broadcast(128)), dst=bpa)
            GM0, CW0, CB0, SCW0, NW0 = 0, 8, 88, 108, 120
            DTB0, AL0, DSK0 = 0, 24, 48
            S.op("act", lambda e: e.activation(out=A_bc[:, :], in_=bpa[:, AL0:AL0 + NH], func=AF.Exp), r=[bpa], w=[A_bc])
            S.op("dve", lambda e: e.tensor_scalar(out=A_bc[:, :], in0=A_bc[:, :], scalar1=-1.0, scalar2=None, op0=ALU.mult),
                 r=[A_bc], w=[A_bc])
            with ExitStack() as est:
                stg = [sb(est, "stg%d" % i, [128, 2828], F32) for i in range(4)]
                n = 0
                for k in range(8):
                    for hb in range(2):
                        st = stg[n % 4]
                        S.dma("sp", lambda e, st=st, k=k, hb=hb: e.dma_start(
                            out=st[:, :], in_=win_d[k * 128:(k + 1) * 128, hb * 2828:(hb + 1) * 2828]), dst=st)
                        if n % 2 == 0:
                            S.op("act", lambda e, st=st, k=k, hb=hb: e.activation(
                                out=win[:, k, hb * 2828:(hb + 1) * 2828], in_=st[:, :], func=AF.Copy,
                                scale=pp[:, GM0 + k:GM0 + k + 1]), r=[st], w=[win], c=[pp])
                        else:
                            S.op("dve", lambda e, st=st, k=k, hb=hb: e.tensor_scalar(
                                out=win[:, k, hb * 2828:(hb + 1) * 2828], in0=st[:, :],
                                scalar1=pp[:, GM0 + k:GM0 + k + 1], scalar2=None, op0=ALU.mult), r=[st], w=[win], c=[pp])
                        n += 1
                S.barrier()
            xts = [sb(ea, "xt%d" % i, [128, D], F32) for i in range(1)]
            xn = sb(ea, "xn", [128, D], BF16)
            junk2 = sb(ea, "junk2", [128, 384], BF16)
            sm = sb(ea, "sm", [128, 4], F32)
            smk = sb(ea, "smk", [128, 16], F32)
            nT = sb(ea, "nT", [128, 8, 128], BF16)
            xpres = [sb(ea, "xpre%d" % i, [128, 20, 131], BF16) for i in range(2)]
            hxs = [sb(ea, "hx%d" % i, [128, 20, 3], BF16) for i in range(3)]
            hcs = [sb(ea, "hc%d" % i, [128, 4, 2], F32) for i in range(3)]
            xacts = [sb(ea, "xact%d" % i, [128, 20, 128], BF16) for i in range(2)]
            cacc = [sb(ea, "cacc%d" % i, [128, 128], F32) for i in range(8)]
            scbs = [sb(ea, "scb%d" % i, [128, 4, 128], BF16) for i in range(2)]
            scc = sb(ea, "scc", [128, 4, 128], F32)
            cxs = [sb(ea, "cx%d" % i, [128, 4, 130], F32) for i in range(2)]
            sacc = sb(ea, "sacc", [128, 4, 128], F32)
            szs = [sb(ea, "sz%d" % i, [128, DSSD], BF16) for i in range(3)]
            xsB = sb(ea, "xsB", [128, 2048], BF16)
            xdt = sb(ea, "xdt", [128, DSSD], BF16)
            xdd = sb(ea, "xdd", [128, DSSD], BF16)
            dtfs = [sb(ea, "dtf%d" % i, [128, 3, NH], F32) for i in range(3)]
            dts = sb(ea, "dts", [128, 8, NH], F32)
            rhsA = [sb(ea, "rhsA%d" % i, [128, 384], F32) for i in range(2)]
            Dm = [sb(ea, "Dm%d" % i, [128, 384], F32) for i in range(2)]
            MT = [sb(ea, "MT%d" % i, [128, 384], BF16) for i in range(3)]
            cbT = sb(ea, "cbT", [128, 512], F32)
            t1 = sb(ea, "t1", [128, DSSD], F32)
            yn = sb(ea, "yn", [128, DSSD], BF16)
            YTs = [sb(ea, "YT%d" % i, [128, 16, 128], BF16) for i in range(2)]
            St = sb(ea, "St", [128, DSSD], F32)
            Stmp = sb(ea, "Stmp", [128, DSSD], F32)
            t1b = sb(ea, "t1b", [128, DSSD], BF16)
            yb = t1
            Sbf = sb(ea, "Sbf", [128, DSSD], BF16)
            Smeta = sb(ea, "Smeta", [128, DSSD], F32)
            halo_x = sb(ea, "halo_x", [128, 20, 3], F32)
            halo_c = sb(ea, "halo_c", [128, 4, 2], F32)
            state = {"n": 0}

            def frontA(seq, ti, T, meta, par, sj=0, jg=0):
                first = (ti == 0) and not meta
                last = (ti == ntile - 1) and not meta
                xpre = xpres[jg % 2]
                cx = cxs[jg % 2]
                scb = scbs[jg % 2]
                junk = xn
                xt = xts[0]
                YT = YTs[par]
                xact = xacts[par]
                sz = szs[sj]
                dtf = dtfs[sj]
                pool = fpool
                src = meta_d.ap() if meta else x_d[seq, ti * 128:(ti + 1) * 128, :]
                S.dma("sp", lambda e: e.dma_start(out=xt[0:T, :], in_=src), dst=xt)
                S.op("act", lambda e: e.activation(out=junk[0:T, 0:D], in_=xt[0:T, :], func=AF.Square,
                                                   accum_out=sm[0:T, 0:1]), r=[xt], w=[xn, sm])
                S.op("act", lambda e: e.activation(out=sm[0:T, 1:2], in_=sm[0:T, 0:1], func=AF.Ln, scale=1.0 / D, bias=EPS),
                     r=[sm], w=[sm])
                S.op("act", lambda e: e.activation(out=sm[0:T, 2:3], in_=sm[0:T, 1:2], func=AF.Exp, scale=-0.5),
                     r=[sm], w=[sm])
                S.op("act", lambda e: e.activation(out=xn[0:T, :], in_=xt[0:T, :], func=AF.Copy, scale=sm[0:T, 2:3]),
                     r=[xt, sm], w=[xn])
                yield
                pt = pool.get()
                ptb = pt.t[:].bitcast(BF16)
                for k in range(8):
                    S.op("pe", lambda e, k=k: e.transpose(out=ptb[:, k * 128:k * 128 + T], in_=xn[0:T, k * 128:(k + 1) * 128],
                                                          identity=identb[0:T, 0:T]), r=[xn], w=[pt], c=[identb])
                yield
                S.op("dve", lambda e: e.tensor_copy(out=nT[:, :, 0:T], in_=ptb.rearrange("p (k t) -> p k t", k=8)[:, :, 0:T]),
                     r=[pt], w=[nT])
                yield

                fmres = {}

                def fm_bank(col0, nchunk=4):
                    p = pool.get()
                    fmres["p"] = p
                    for j in range(nchunk):
                        for k in range(8):
                            S.op("pe", lambda e, j=j, k=k: e.matmul(
                                p[:, j * 128:j * 128 + T], lhsT=win[:, k, col0 + j * 128:col0 + (j + 1) * 128],
                                rhs=nT[:, k, 0:T], start=(k == 0), stop=(k == 7)), r=[nT], w=[p], c=[win])
                        yield

                def p3(p):
                    return p.t[:].rearrange("p (j t) -> p j t", j=4)[:, :, 0:T]

                for bq in range(5):
                    yield from fm_bank(C_XBC + bq * 512)
                    p = fmres["p"]
                    eng = "act" if bq % 2 == 0 else "dve"
                    if eng == "act":
                        S.op("act", lambda e, p=p, bq=bq: e.activation(out=xpre[:, 4 * bq:4 * bq + 4, 3:3 + T], in_=p3(p), func=AF.Copy),
                             r=[p], w=[xpre])
                    else:
                        S.op("dve", lambda e, p=p, bq=bq: e.tensor_copy(out=xpre[:, 4 * bq:4 * bq + 4, 3:3 + T], in_=p3(p)),
                             r=[p], w=[xpre])
                    yield
                if not meta:
                    yield from fm_bank(C_SCB)
                    p = fmres["p"]
                    S.op("act", lambda e, p=p: e.activation(out=scb[:, :, 0:T], in_=p3(p), func=AF.Copy), r=[p], w=[scb])
                yield from fm_bank(C_SCC)
                p = fmres["p"]
                S.op("act", lambda e, p=p: e.activation(out=scc[:, :, 0:T], in_=p3(p), func=AF.Copy), r=[p], w=[scc])
                yield from fm_bank(C_SCX)
                p = fmres["p"]
                S.op("dve", lambda e, p=p: e.tensor_tensor(out=cx[:, :, 2:2 + T], in0=scc[:, :, 0:T], in1=p3(p), op=ALU.mult),
                     r=[p, scc], w=[cx])
                yield
                p = pool.get()
                for k in range(8):
                    S.op("pe", lambda e, k=k, p=p: e.matmul(p[0:T, 0:NH], lhsT=nT[:, k, 0:T], rhs=win[:, k, C_DT:C_DT + NH],
                                                            start=(k == 0), stop=(k == 7)), r=[nT], w=[p], c=[win])
                S.op("dve", lambda e, p=p: e.tensor_tensor(out=dtf[0:T, 2, :], in0=p[0:T, 0:NH], in1=bpa[0:T, DTB0:DTB0 + NH], op=ALU.add),
                     r=[p], w=[dtf], c=[bpa])
                S.op("act", lambda e: e.activation(out=dtf[0:T, 2, :], in_=dtf[0:T, 2, :], func=AF.Exp), r=[dtf], w=[dtf])
                S.op("act", lambda e: e.activation(out=dtf[0:T, 0, :], in_=dtf[0:T, 2, :], func=AF.Ln, bias=1.0), r=[dtf], w=[dtf])
                S.op("dve", lambda e: e.tensor_tensor(out=dtf[0:T, 1, :], in0=dtf[0:T, 0, :], in1=A_bc[0:T, :], op=ALU.mult),
                     r=[dtf], w=[dtf], c=[A_bc])
                yield
                if not meta:
                    for zb in range(3):
                        p = pool.get()
                        for k in range(8):
                            S.op("pe", lambda e, k=k, p=p, zb=zb: e.matmul(
                                p[0:T, :], lhsT=nT[:, k, 0:T], rhs=win[:, k, C_Z + zb * 512:C_Z + (zb + 1) * 512],
                                start=(k == 0), stop=(k == 7)), r=[nT], w=[p], c=[win])
                            if k % 4 == 3:
                                yield
                        S.op("act", lambda e, p=p, zb=zb: e.activation(out=sz[0:T, zb * 512:(zb + 1) * 512], in_=p[0:T, :], func=AF.Silu),
                             r=[p], w=[sz])
                        yield
                if not meta and not last:
                    S.op("act", lambda e: e.activation(out=hxs[sj][:, :, :], in_=xpre[:, :, T:T + 3], func=AF.Copy), r=[xpre], w=[hxs[sj]])
                    S.op("act", lambda e: e.activation(out=hcs[sj][:, :, :], in_=cx[:, :, T:T + 2], func=AF.Copy), r=[cx], w=[hcs[sj]])
                yield "mid"
                if meta:
                    S.op("pool", lambda e: e.memset(xpre[:, :, 0:3], 0.0), w=[xpre])
                    S.op("pool", lambda e: e.memset(cx[:, :, 0:2], 0.0), w=[cx])
                elif first:
                    S.op("act", lambda e: e.activation(out=xpre[:, :, 0:3], in_=halo_x[:, :, :], func=AF.Copy), r=[halo_x], w=[xpre])
                    S.op("act", lambda e: e.activation(out=cx[:, :, 0:2], in_=halo_c[:, :, :], func=AF.Copy), r=[halo_c], w=[cx])
                else:
                    hp = (sj + 2) % 3
                    S.op("act", lambda e: e.activation(out=xpre[:, :, 0:3], in_=hxs[hp][:, :, :], func=AF.Copy), r=[hxs[hp]], w=[xpre])
                    S.op("act", lambda e: e.activation(out=cx[:, :, 0:2], in_=hcs[hp][:, :, :], func=AF.Copy), r=[hcs[hp]], w=[cx])
                def conv_head(g):
                    for j in range(4):
                        k = 4 * g + j
                        ca = cacc[k % 8]
                        S.op("act", lambda e, k=k, ca=ca: e.activation(
                            out=ca[:, 0:T], in_=xpre[:, k, 3:3 + T], func=AF.Identity,
                            scale=pp[:, CW0 + 4 * k + 3:CW0 + 4 * k + 4], bias=pp[:, CB0 + k:CB0 + k + 1]), r=[xpre], w=[ca], c=[pp])

                def conv_taps(g):
                    for w_ in range(3):
                        for j in range(4):
                            k = 4 * g + j
                            ca = cacc[k % 8]
                            S.op("dve", lambda e, k=k, ca=ca, w_=w_: e.scalar_tensor_tensor(
                                out=ca[:, 0:T], in0=xpre[:, k, w_:w_ + T], scalar=pp[:, CW0 + 4 * k + w_:CW0 + 4 * k + w_ + 1],
                                in1=ca[:, 0:T], op0=ALU.mult, op1=ALU.add), r=[xpre, ca], w=[ca], c=[pp])

                def conv_tail(g):
                    for j in range(4):
                        k = 4 * g + j
                        ca = cacc[k % 8]
                        S.op("act", lambda e, k=k, ca=ca: e.activation(out=xact[:, k, 0:T], in_=ca[:, 0:T], func=AF.Silu), r=[ca], w=[xact], disjoint=True)

                conv_head(0)
                yield
                for g in range(5):
                    if g + 1 < 5:
                        conv_head(g + 1)
                    conv_taps(g)
                    yield
                    conv_tail(g)
                    yield
                for k in range(4):
                    S.op("act", lambda e, k=k: e.activation(
                        out=sacc[:, k, 0:T], in_=cx[:, k, 2:2 + T], func=AF.Copy, scale=pp[:, SCW0 + 3 * k + 2:SCW0 + 3 * k + 3]),
                        r=[cx], w=[sacc], c=[pp])
                    for w_ in range(2):
                        S.op("dve", lambda e, k=k, w_=w_: e.scalar_tensor_tensor(
                            out=sacc[:, k, 0:T], in0=cx[:, k, w_:w_ + T], scalar=pp[:, SCW0 + 3 * k + w_:SCW0 + 3 * k + w_ + 1],
                            in1=sacc[:, k, 0:T], op0=ALU.mult, op1=ALU.add), r=[cx, sacc], w=[sacc], c=[pp])
                if not meta:
                    S.op("pool", lambda e: e.tensor_tensor(out=YT[:, 12:16, 0:T], in0=sacc[:, :, 0:T], in1=scb[:, :, 0:T], op=ALU.mult),
                         r=[sacc, scb], w=[YT])
                yield
                if meta:
                    S.op("pool", lambda e: e.tensor_copy(out=halo_x[:, :, :], in_=xpre[:, :, T:T + 3]), r=[xpre], w=[halo_x])
                    S.op("pool", lambda e: e.tensor_copy(out=halo_c[:, :, :], in_=cx[:, :, T:T + 2]), r=[cx], w=[halo_c])
            def backA(seq, ti, T, meta, par, pump, sj=0):
                first = (ti == 0) and not meta
                last = (ti == ntile - 1) and not meta
                YT = YTs[par]
                xact = xacts[par]
                sz = szs[sj]
                dtf = dtfs[sj]
                pool = bpool
                for hb in range(2):
                    pt = pool.get()
                    ptb = pt.t[:].bitcast(BF16)
                    for j in range(8):
                        cc = hb * 8 + j
                        S.op("pe", lambda e, j=j, cc=cc, ptb=ptb, pt=pt: e.transpose(
                            out=ptb[0:T, j * 128:(j + 1) * 128], in_=xact[:, cc, 0:T], identity=identb[:, :]),
                            r=[xact], w=[pt], c=[identb])
                    if hb == 0:
                        S.op("act", lambda e, ptb=ptb, pt=pt: e.activation(out=xsB[0:T, 0:1024], in_=ptb[0:T, :], func=AF.Copy), r=[pt], w=[xsB])
                    else:
                        S.op("dve", lambda e, ptb=ptb, pt=pt: e.tensor_copy(out=xsB[0:T, 1024:2048], in_=ptb[0:T, :]), r=[pt], w=[xsB])
                pump(2)
                p = pool.get()
                S.op("pe", lambda e, p=p: e.matmul(p[0:T, 0:NH], lhsT=cst[0:T, TRIU0:TRIU0 + T], rhs=dtf[0:T, 1, :], start=True, stop=True),
                     r=[dtf], w=[p], c=[cst])
                S.op("pe", lambda e, p=p: e.matmul(p[0:T, 32:32 + NH], lhsT=cst[0:T, ONES0:ONES0 + T], rhs=dtf[0:T, 1, :], start=True, stop=True),
                     r=[dtf], w=[p], c=[cst])
                S.op("dve", lambda e, p=p: e.tensor_copy(out=dts[0:T, 2, :], in_=p[0:T, 0:NH]), r=[p], w=[dts])
                S.op("act", lambda e, p=p: e.activation(out=dts[0:T, 3, :], in_=p[0:T, 0:NH], func=AF.Exp), r=[p], w=[dts])
                S.op("act", lambda e, p=p: e.activation(out=dts[0:T, 5, :], in_=p[0:T, 32:32 + NH], func=AF.Exp), r=[p], w=[dts])
                S.op("dve", lambda e, p=p: e.tensor_tensor(out=dts[0:T, 6, :], in0=p[0:T, 32:32 + NH], in1=dts[0:T, 2, :], op=ALU.subtract),
                     r=[p, dts], w=[dts])
                S.op("act", lambda e: e.activation(out=dts[0:T, 4, :], in_=dts[0:T, 6, :], func=AF.Exp), r=[dts], w=[dts])

                pump(2)

                def hb3(ap2, nh=NH):
                    return ap2.rearrange("p (h q) -> p h q", h=nh)

                def bch(ap2, nh=NH):
                    return ap2.unsqueeze(2).to_broadcast([T, nh, HP])

                S.op("dve", lambda e: e.tensor_tensor(out=hb3(xdt[0:T, :]), in0=hb3(xsB[0:T, 0:DSSD]), in1=bch(dtf[0:T, 0, :]), op=ALU.mult),
                     r=[xsB, dtf], w=[xdt])
                S.op("pool", lambda e: e.tensor_tensor(out=hb3(xdd[0:T, :]), in0=hb3(xdt[0:T, :]), in1=bch(dts[0:T, 4, :]), op=ALU.mult),
                     r=[xdt, dts], w=[xdd])
                if not meta:
                    for g in range(NG):
                        p = pool.get()
                        S.op("pe", lambda e, p=p, g=g: e.matmul(p[0:T, 0:384], lhsT=xact[:, 16 + g, 0:T], rhs=Sbf[:, g * 384:(g + 1) * 384],
                                                                start=True, stop=True), r=[xact, Sbf], w=[p])
                        S.op("dve", lambda e, p=p, g=g: e.tensor_tensor(
                            out=hb3(t1[0:T, g * 384:(g + 1) * 384], 6), in0=hb3(p[0:T, 0:384], 6),
                            in1=bch(dts[0:T, 3, 6 * g:6 * g + 6], 6), op=ALU.mult), r=[p, dts], w=[t1], disjoint=True)
                        pump(2)
                    S.op("pool", lambda e: e.tensor_tensor(out=hb3(t1b[0:T, :]), in0=hb3(xsB[0:T, 0:DSSD]),
                                                           in1=bch(bpa[0:T, DSK0:DSK0 + NH]), op=ALU.mult), r=[xsB], w=[t1b], c=[bpa])
                    S.op("dve", lambda e: e.tensor_tensor(out=t1[0:T, :], in0=t1[0:T, :], in1=t1b[0:T, :], op=ALU.add), r=[t1, t1b], w=[t1])
                if not last:
                    dstS = Smeta if meta else St
                    if not meta:
                        S.op("pool", lambda e: e.tensor_tensor(out=hb3(Stmp[:, :]), in0=hb3(St[:, :]),
                                                               in1=dts[:, 5, :].unsqueeze(2).to_broadcast([128, NH, HP]), op=ALU.mult),
                             r=[St, dts], w=[Stmp])
                    for g in range(NG):
                        p = pool.get()
                        S.op("pe", lambda e, p=p, g=g: e.matmul(p[:, 0:384], lhsT=xsB[0:T, DSSD + g * 128:DSSD + (g + 1) * 128],
                                                                rhs=xdd[0:T, g * 384:(g + 1) * 384], start=True, stop=True),
                             r=[xsB, xdd], w=[p])
                        if meta:
                            S.op("dve", lambda e, g=g, p=p: e.tensor_copy(out=dstS[:, g * 384:(g + 1) * 384], in_=p[:, 0:384]),
                                 r=[p], w=[dstS])
                        else:
                            S.op("dve", lambda e, g=g, p=p: e.tensor_tensor(out=St[:, g * 384:(g + 1) * 384], in0=p[:, 0:384],
                                                                            in1=Stmp[:, g * 384:(g + 1) * 384], op=ALU.add),
                                 r=[p, Stmp], w=[St], disjoint=True)
                        pump(2)
                    if not meta:
                        S.op("act", lambda e: e.activation(out=Sbf[:, :], in_=St[:, :], func=AF.Copy), r=[St], w=[Sbf])
                if not meta:
                    p = pool.get()
                    for g in range(NG):
                        S.op("pe", lambda e, p=p, g=g: e.matmul(p[0:T, g * 128:g * 128 + T], lhsT=xact[:, 12 + g, 0:T], rhs=xact[:, 16 + g, 0:T],
                                                                start=True, stop=True), r=[xact], w=[p])
                    S.op("act", lambda e, p=p: e.activation(out=cbT[0:T, :], in_=p[0:T, :], func=AF.Copy), r=[p], w=[cbT])
                    pump(2)
                    ybanks = ps[5:8]
                    dbank = {}

                    def unit_head(u):
                        ra = rhsA[u % 2]
                        for j in range(3):
                            h = 3 * u + j
                            S.op("act", lambda e, j=j, h=h: e.activation(
                                out=ra[0:T, j * T:(j + 1) * T], in_=cst[0:T, TRIU0:TRIU0 + T], func=AF.Copy,
                                scale=dtf[0:T, 1, h:h + 1]), r=[dtf], w=[ra], c=[cst], disjoint=True)
                        p = pool.get()
                        dbank[u] = p
                        S.op("pe", lambda e: e.matmul(p[0:T, 0:3 * T], lhsT=cst[0:T, ONES0:ONES0 + T], rhs=ra[0:T, 0:3 * T],
                                                      start=True, stop=True), r=[ra], w=[p], c=[cst])

                    unit_head(0)
                    pump(2)
                    for u in range(8):
                        g = u // 2
                        dm = Dm[u % 2]
                        mt = MT[u % 3]
                        if u + 1 < 8:
                            unit_head(u + 1)
                        p = dbank[u]
                        for j in range(3):
                            h = 3 * u + j
                            S.op("dve", lambda e, p=p, dm=dm, j=j, h=h: e.scalar_tensor_tensor(
                                out=dm[0:T, j * T:(j + 1) * T], in0=p[0:T, j * T:(j + 1) * T], scalar=dts[0:T, 2, h:h + 1],
                                in1=cst[0:T, NEGM0:NEGM0 + T], op0=ALU.subtract, op1=ALU.add), r=[p, dts], w=[dm], c=[cst], disjoint=True)
                        pump(2)
                        S.op("act", lambda e, dm=dm: e.activation(out=dm[0:T, 0:3 * T], in_=dm[0:T, 0:3 * T], func=AF.Exp), r=[dm], w=[dm])
                        pump(2)
                        S.op("dve", lambda e, dm=dm, mt=mt, g=g: e.tensor_tensor(
                            out=mt[0:T, 0:3 * T].rearrange("p (j l) -> p j l", j=3),
                            in0=dm[0:T, 0:3 * T].rearrange("p (j l) -> p j l", j=3),
                            in1=cbT[0:T, g * 128:g * 128 + T].unsqueeze(1).to_broadcast([T, 3, T]), op=ALU.mult),
                            r=[dm, cbT], w=[mt])
                        for j in range(3):
                            h = 3 * u + j
                            yp = ybanks[h // 8]
                            S.op("pe", lambda e, yp=yp, mt=mt, j=j, h=h: e.matmul(
                                yp[0:T, (h % 8) * 64:(h % 8 + 1) * 64], lhsT=mt[0:T, j * T:(j + 1) * T],
                                rhs=xdt[0:T, h * 64:(h + 1) * 64], start=True, stop=True), r=[mt, xdt], w=[yp])
                        pump(2)
                    pump.mid()
                    for bq in range(3):
                        S.op("dve", lambda e, bq=bq: e.tensor_tensor(out=yb[0:T, bq * 512:(bq + 1) * 512], in0=ybanks[bq][0:T, :],
                                                                     in1=t1[0:T, bq * 512:(bq + 1) * 512], op=ALU.add),
                             r=[ybanks[bq], t1], w=[yb])
                        pump(2)
                    S.op("dve", lambda e: e.tensor_tensor(out=yb[0:T, :], in0=yb[0:T, :], in1=sz[0:T, :], op=ALU.mult), r=[yb, sz], w=[yb])
                    for g in range(NG):
                        S.op("act", lambda e, g=g: e.activation(out=junk2[0:T, 0:384], in_=yb[0:T, g * 384:(g + 1) * 384], func=AF.Square,
                                                                accum_out=smk[0:T, 4 + g:5 + g]), r=[yb], w=[smk, junk2])
                    S.op("act", lambda e: e.activation(out=smk[0:T, 8:12], in_=smk[0:T, 4:8], func=AF.Ln, scale=1.0 / 384, bias=EPS), r=[smk], w=[smk])
                    S.op("act", lambda e: e.activation(out=smk[0:T, 12:16], in_=smk[0:T, 8:12], func=AF.Exp, scale=-0.5), r=[smk], w=[smk])
                    S.op("dve", lambda e: e.tensor_tensor(
                        out=yn[0:T, :].rearrange("p (g q) -> p g q", g=NG), in0=yb[0:T, :].rearrange("p (g q) -> p g q", g=NG),
                        in1=smk[0:T, 12:16].unsqueeze(2).to_broadcast([T, NG, 384]), op=ALU.mult), r=[yb, smk], w=[yn])
                    pump(2)
                    for hb in range(2):
                        nchk = 8 if hb == 0 else 4
                        pt = pool.get()
                        ptb = pt.t[:].bitcast(BF16)
                        for j in range(nchk):
                            cc = hb * 8 + j
                            S.op("pe", lambda e, j=j, cc=cc, ptb=ptb: e.transpose(
                                out=ptb[:, j * 128:j * 128 + T], in_=yn[0:T, cc * 128:(cc + 1) * 128], identity=identb[0:T, 0:T]),
                                r=[yn], w=[pt], c=[identb])
                        S.op("act", lambda e, ptb=ptb, hb=hb, nchk=nchk: e.activation(
                            out=YT[:, hb * 8:hb * 8 + nchk, 0:T], in_=ptb.rearrange("p (k t) -> p k t", k=8)[:, 0:nchk, 0:T], func=AF.Copy),
                            r=[pt], w=[YT])
                        pump(2)
                    S.dma("pool", lambda e: e.dma_start(out=yt_d[seq * NT + ti, :, :], in_=YT[:, :, :].rearrange("p k t -> p (k t)")),
                          dst=yt_b, r=[YT])
                pump(10 ** 6)

            def dump(nm, tl, ap):
                bb = Buf("dbg_" + nm)
                dbg_bufs.append(bb)
                S.dma("pool", lambda e: e.dma_start(out=dbg_d[nm].ap(), in_=ap), dst=bb, r=[tl])

            class FrontRun:
                def __init__(self, gen):
                    self.g = gen
                    self.done = gen is None
                    self.at_mid = False

                def step(self, n, stop_mid=False):
                    for _ in range(n):
                        if self.done or (stop_mid and self.at_mid):
                            return
                        try:
                            v = next(self.g)
                            if v == "mid":
                                self.at_mid = True
                        except StopIteration:
                            self.done = True

                def flush(self):
                    while not self.done:
                        self.step(1000)

            class Pumper:
                def __init__(self, fa, fb):
                    self.fa, self.fb = fa, fb

                def __call__(self, n=1):
                    if n >= 10 ** 6:
                        if self.fa is not None:
                            self.fa.flush()
                        if self.fb is not None:
                            self.fb.step(10 ** 6, stop_mid=True)
                        return
                    for _ in range(n):
                        if self.fa is not None:
                            self.fa.step(1)
                        if self.fb is not None:
                            self.fb.step(2, stop_mid=True)

                def mid(self):
                    pass

            nopump = Pumper(None, None)
            for _ in frontA(0, 0, NMETA, True, 0):
                pass
            backA(0, 0, NMETA, True, 0, nopump)
            if dbgA:
                dump("Smeta", Smeta, Smeta[:, :])
            tiles = [(sq, ti) for sq in range(nseq) for ti in range(ntile)]
            runs = {}

            def get_run(j):
                if j >= len(tiles):
                    return None
                if j not in runs:
                    runs[j] = FrontRun(frontA(tiles[j][0], tiles[j][1], 128, False, (j + 1) % 2, sj=j % 3, jg=j))
                return runs[j]

            get_run(0).flush()
            if get_run(1) is not None:
                get_run(1).step(10 ** 6, stop_mid=True)
            for i, (sq, ti) in enumerate(tiles):
                par = (i + 1) % 2
                if ti == 0:
                    S.op("pool", lambda e: e.tensor_copy(out=St[:, :], in_=Smeta[:, :]), r=[Smeta], w=[St])
                    S.op("act", lambda e: e.activation(out=Sbf[:, :], in_=Smeta[:, :], func=AF.Copy), r=[Smeta], w=[Sbf])
                uv_issue(1)
                pump = Pumper(get_run(i + 1), get_run(i + 2))
                backA(sq, ti, 128, False, par, pump, sj=i % 3)
                if dbgA and sq == 0 and ti == 0:
                    dump("dts", dts, dts[:, :, :].rearrange("p a h -> p (a h)"))
                    dump("xsB", xsB, xsB[:, :]); dump("xdt", xdt, xdt[:, :]); dump("cbT", cbT, cbT[:, :])
                    dump("t1", t1, t1[:, :]); dump("yb", yb, yb[:, :]); dump("St", St, St[:, :]); dump("sz", szs[i % 3], szs[i % 3][:, :])
                    dump("Dm1", Dm[1], Dm[1][:, :]); dump("MT1", MT[1], MT[1][:, :])
                    dump("xact", xacts[par], xacts[par][:, :, :].rearrange("p k t -> p (k t)"))
            uv_issue(10 ** 6)
            S.barrier()

        if dbgA:
            S.final_wait("sp", [yt_b] + dbg_bufs)
            return nc
        with ExitStack() as eb:
            gpool = Pool(ps[0:4])
            ffnbs = [[ps[4], ps[5]], [ps[6], ps[7]]]
            wout = sb(eb, "wout", [128, 16, D], BF16)
            wq = sb(eb, "wq", [128, 8, 2048], BF16)
            skt = sb(eb, "skt", [128, 16, 128], BF16)
            bpb = sb(eb, "bpb", [128, 2048], F32)
            ppb = sb(eb, "ppb", [128, 136], F32)
            NW0 = 120
            S.dma("sp", lambda e: e.dma_start(out=bpb[:, :], in_=bpb_d.ap().partition_broadcast(128)), dst=bpb)
            S.dma("sp", lambda e: e.dma_start(out=ppb[:, :], in_=pp_d.ap()), dst=ppb)
            with ExitStack() as est:
                stg = [sb(est, "stgb%d" % i, [128, 2048], F32) for i in range(4)]
                n = 0
                for c in range(16):
                    st = stg[n % 4]
                    S.dma("sp", lambda e, st=st, c=c: e.dma_start(out=st[:, 0:D], in_=wout_d[c * 128:(c + 1) * 128, :]), dst=st)
                    eng = "act" if n % 2 == 0 else "dve"
                    if eng == "act":
                        S.op("act", lambda e, st=st, c=c: e.activation(out=wout[:, c, :], in_=st[:, 0:D], func=AF.Copy,
                                                                       scale=ppb[:, NW0 + c:NW0 + c + 1]), r=[st], w=[wout], c=[ppb])
                    else:
                        S.op("dve", lambda e, st=st, c=c: e.tensor_scalar(out=wout[:, c, :], in0=st[:, 0:D], scalar1=ppb[:, NW0 + c:NW0 + c + 1],
                                                                          scalar2=None, op0=ALU.mult), r=[st], w=[wout], c=[ppb])
                    n += 1
                for k in range(8):
                    st = stg[n % 4]
                    S.dma("sp", lambda e, st=st, k=k: e.dma_start(out=st[:, :], in_=wq_d[k * 128:(k + 1) * 128, :]), dst=st)
                    if n % 2 == 0:
                        S.op("act", lambda e, st=st, k=k: e.activation(out=wq[:, k, :], in_=st[:, :], func=AF.Copy), r=[st], w=[wq])
                    else:
                        S.op("dve", lambda e, st=st, k=k: e.tensor_copy(out=wq[:, k, :], in_=st[:, :]), r=[st], w=[wq])
                    n += 1
                st = stg[n % 4]
                S.dma("sp", lambda e, st=st: e.dma_start(out=st[:, :], in_=skt_d.ap()), dst=st)
                S.op("dve", lambda e, st=st: e.tensor_copy(out=skt[:, :, :].rearrange("p j k -> p (j k)"), in_=st[:, :]), r=[st], w=[skt])
                S.barrier()
            GF0, GL0 = 0, 1024
            ytl = [sb(eb, "ytl%d" % i, [128, 16, 128], BF16) for i in range(2)]
            xtb = [sb(eb, "xtb%d" % i, [128, D], F32) for i in range(2)]
            h1s = [sb(eb, "h1_%d" % i, [128, D], F32) for i in range(2)]
            xn2s = [sb(eb, "xn2_%d" % i, [128, D], BF16) for i in range(2)]
            idss = [sb(eb, "ids%d" % i, [128, NSLOT], I32) for i in range(2)]
            gatess = [sb(eb, "gates%d" % i, [128, NSLOT], F32) for i in range(2)]
            junkb = sb(eb, "junkb", [128, D], BF16)
            smb = sb(eb, "smb", [128, 16], F32)
            smo = sb(eb, "smo", [128, 4], F32)
            mhalf = sb(eb, "mhalf", [128, 1], F32)
            S.op("pool", lambda e: e.memset(mhalf[:, :], -0.5), w=[mhalf])
            n2T = sb(eb, "n2T", [128, 8, 128], BF16)
            qT = sb(eb, "qT", [128, 16, 128], BF16)
            ssb = sb(eb, "ssb", [128, 16, 128], F32)
            ss2 = sb(eb, "ss2", [128, 16, 128], F32)
            vv = sb(eb, "vv", [128, 16, 16], F32)
            ixu = sb(eb, "ixu", [128, 16, 16], U32)
            ixf = sb(eb, "ixf", [128, 16, 16], F32)
            cand = Tl(ssb.t[:, :, :].rearrange("p (h two) k -> p h (two k)", two=2), "cand_alias")
            cand.b = ssb.b
            cand2 = Tl(ss2.t[:, :, :].rearrange("p (h two) k -> p h (two k)", two=2), "cand2_alias")
            cand2.b = ss2.b
            best = sb(eb, "best", [128, 8, 16], F32)
            posu = sb(eb, "posu", [128, 8, 16], U32)
            pa = sb(eb, "pa", [128, 128], U32)
            pb = sb(eb, "pb", [128, 128], U32)
            paf = sb(eb, "paf", [128, 128], F32)
            pbf = sb(eb, "pbf", [128, 128], F32)
            oh = sb(eb, "oh", [128, 128, 16], F32)
            i1 = sb(eb, "i1", [128, 128], F32)
            i2 = sb(eb, "i2", [128, 128], F32)
            ex = sb(eb, "ex", [128, 8, 16], F32)
            hidr = [sb(eb, "hid%d" % i, [128, 4], F32) for i in range(6)]
            UVb = [sb(eb, "UVb%d" % i, [128, 2 * D], BF16) for i in range(NGB)]
            dg = [sb(eb, "dg%d" % i, [128, 128], BF16) for i in range(6)]
            ot = [sb(eb, "ot%d" % i, [128, D], F32) for i in range(2)]
            junkfs = [sb(eb, "junkf%d" % i, [128, D], BF16) for i in range(3)]

            vvB = [Buf("vv%d" % j) for j in range(16)]
            ixB = [Buf("ix%d" % j) for j in range(16)]
            s2B = [Buf("s2_%d" % j) for j in range(16)]
            beB = [Buf("be%d" % h) for h in range(8)]
            poB = [Buf("po%d" % h) for h in range(8)]
            c2B = [Buf("c2_%d" % h) for h in range(8)]

            def stageA(it):
                seq, ti = divmod(it, ntile)
                par = it % 2
                yl, xt, h1, xn2, ids, gates = ytl[par], xtb[par], h1s[par], xn2s[par], idss[par], gatess[par]
                S.dma("sp", lambda e: e.dma_start(out=yl[:, :, :].rearrange("p k t -> p (k t)"), in_=yt_d[seq * NT + ti, :, :]),
                      dst=yl, r=[yt_b])
                S.dma("sp", lambda e: e.dma_start(out=xt[:, :], in_=x_d[seq, ti * 128:(ti + 1) * 128, :]), dst=xt)
                yield
                for nb in range(2):
                    p = gpool.get()
                    for c in range(16):
                        S.op("pe", lambda e, p=p, c=c, nb=nb: e.matmul(p[:, :], lhsT=yl[:, c, :], rhs=wout[:, c, nb * 512:(nb + 1) * 512],
                                                                       start=(c == 0), stop=(c == 15)), r=[yl], w=[p], c=[wout])
                        if c % 2 == 1:
                            yield
                    S.op("dve", lambda e, p=p, nb=nb: e.tensor_tensor(out=h1[:, nb * 512:(nb + 1) * 512], in0=p[:, :],
                                                                      in1=xt[:, nb * 512:(nb + 1) * 512], op=ALU.add), r=[p, xt], w=[h1])
                    yield
                S.op("act", lambda e: e.activation(out=junkb[:, :], in_=h1[:, :], func=AF.Square, accum_out=smb[:, 0:1]), r=[h1], w=[smb, junkb])
                S.op("pool", lambda e: e.tensor_scalar(out=smb[:, 1:2], in0=smb[:, 0:1], scalar1=1.0 / D, scalar2=EPS, op0=ALU.mult, op1=ALU.add),
                     r=[smb], w=[smb])
                S.op("pool", lambda e: e.tensor_tensor(out=smb[:, 2:3], in0=smb[:, 1:2], in1=mhalf[:, 0:1], op=ALU.pow), r=[smb], w=[smb], c=[mhalf])
                yield
                S.op("dve", lambda e: e.scalar_tensor_tensor(out=xn2[:, :], in0=h1[:, :], scalar=smb[:, 2:3], in1=bpb[:, GF0:GF0 + D],
                                                             op0=ALU.mult, op1=ALU.mult), r=[h1, smb], w=[xn2], c=[bpb])
                yield
                pt = gpool.get()
                ptb = pt.t[:].bitcast(BF16)
                for k in range(8):
                    S.op("pe", lambda e, k=k: e.transpose(out=ptb[:, k * 128:(k + 1) * 128], in_=xn2[:, k * 128:(k + 1) * 128],
                                                          identity=identb[:, :]), r=[xn2], w=[pt], c=[identb])
                yield
                S.op("act", lambda e: e.activation(out=n2T[:, :, :].rearrange("p k t -> p (k t)"), in_=ptb[:, :], func=AF.Copy), r=[pt], w=[n2T])
                yield
                for qb in range(4):
                    p = gpool.get()
                    for j in range(4):
                        jj = qb * 4 + j
                        for k in range(8):
                            S.op("pe", lambda e, p=p, j=j, jj=jj, k=k: e.matmul(
                                p[:, j * 128:(j + 1) * 128], lhsT=wq[:, k, jj * 128:(jj + 1) * 128], rhs=n2T[:, k, :],
                                start=(k == 0), stop=(k == 7)), r=[n2T], w=[p], c=[wq])
                        yield
                    S.op("act", lambda e, p=p, qb=qb: e.activation(out=qT[:, 4 * qb:4 * qb + 4, :].rearrange("p j t -> p (j t)"), in_=p[:, :],
                                                                   func=AF.Copy), r=[p], w=[qT])
                    yield
                for qb in range(4):
                    p = gpool.get()
                    for j in range(4):
                        jj = qb * 4 + j
                        S.op("pe", lambda e, p=p, j=j, jj=jj: e.matmul(p[:, j * 128:(j + 1) * 128], lhsT=qT[:, jj, :], rhs=skt[:, jj, :],
                                                                       start=True, stop=True), r=[qT], w=[p], c=[skt])
                    yield
                    S.op("act", lambda e, p=p, qb=qb: e.activation(out=ssb[:, 4 * qb:4 * qb + 4, :].rearrange("p j t -> p (j t)"), in_=p[:, :],
                                                                   func=AF.Copy), r=[p], w=[ssb])
                    yield
                def topk_steps(vals, vals2, vB, iB, v2B, outv, outi, idxs, alias_w=lambda j: []):
                    steps = []
                    for j in idxs:
                        steps.append(lambda j=j: S.op("dve", lambda e: e.max(out=outv[:, j, 0:8], in_=vals[:, j, :]), r=[vals], w=[vB[j]]))
                    for j in idxs:
                        steps.append(lambda j=j: S.op("dve", lambda e: e.max_index(out=outi[:, j, 0:8], in_max=outv[:, j, 0:8], in_values=vals[:, j, :]),
                                                      r=[vals, vB[j]], w=[iB[j]]))
                    for j in idxs:
                        steps.append(lambda j=j: S.op("dve", lambda e: e.match_replace(out=vals2[:, j, :], in_to_replace=outv[:, j, 0:8],
                                                                                        in_values=vals[:, j, :], imm_value=-1e30),
                                                      r=[vals, vB[j]], w=[v2B[j]] + alias_w(j)))
                    for j in idxs:
                        steps.append(lambda j=j: S.op("dve", lambda e: e.max(out=outv[:, j, 8:16], in_=vals2[:, j, :]), r=[v2B[j]], w=[vB[j]]))
                    for j in idxs:
                        steps.append(lambda j=j: S.op("dve", lambda e: e.max_index(out=outi[:, j, 8:16], in_max=outv[:, j, 8:16], in_values=vals2[:, j, :]),
                                                      r=[v2B[j], vB[j]], w=[iB[j]]))
                    return steps

                for j0 in range(0, 16, 4):
                    st = topk_steps(ssb, ss2, vvB, ixB, s2B, vv, ixu, range(j0, j0 + 4), alias_w=lambda j: [c2B[j // 2]])
                    for i_, f_ in enumerate(st):
                        f_()
                        if i_ % 3 == 2:
                            yield
                    yield
                S.op("dve", lambda e: e.tensor_copy(out=ixf[:, :, :], in_=ixu[:, :, :]), r=ixB, w=[ixf])
                v4 = vv[:, :, :].rearrange("p (h two) k -> p h two k", two=2)
                S.op("dve", lambda e: e.tensor_tensor(
                    out=cand[:, :, :].rearrange("p h (a b) -> p h a b", a=16),
                    in0=v4[:, :, 0, :].unsqueeze(3).to_broadcast([128, 8, 16, 16]),
                    in1=v4[:, :, 1, :].unsqueeze(2).to_broadcast([128, 8, 16, 16]), op=ALU.add), r=vvB, w=[cand])
                yield
                for h0 in range(0, 8, 4):
                    st = topk_steps(cand, cand2, beB, poB, c2B, best, posu, range(h0, h0 + 4), alias_w=lambda h: [s2B[2 * h], s2B[2 * h + 1]])
                    for i_, f_ in enumerate(st):
                        f_()
                        if i_ % 3 == 2:
                            yield
                    yield
                S.op("dve", lambda e: e.tensor_tensor(out=ex[:, :, :], in0=best[:, :, :], in1=best[:, :, 0:1].to_broadcast([128, 8, 16]),
                                                      op=ALU.subtract), r=beB, w=[ex])
                yield
                S.op("act", lambda e: e.activation(out=ex[:, :, :], in_=ex[:, :, :], func=AF.Exp), r=[ex], w=[ex])
                yield
                S.op("dve", lambda e: e.tensor_reduce(out=smb[:, 4:12], in_=ex[:, :, :], axis=AX.X, op=ALU.add), r=[ex], w=[smb])
                S.op("dve", lambda e: e.reciprocal(out=smb[:, 4:12], in_=smb[:, 4:12]), r=[smb], w=[smb])
                S.op("dve", lambda e: e.tensor_tensor(out=gates[:, :].rearrange("p (h k) -> p h k", h=8), in0=ex[:, :, :],
                                                      in1=smb[:, 4:12].unsqueeze(2).to_broadcast([128, 8, 16]), op=ALU.mult),
                     r=[ex, smb], w=[gates])
                yield
                pos2 = posu[:, :, :].rearrange("p h k -> p (h k)")
                S.op("dve", lambda e: e.tensor_single_scalar(out=pa[:, :], in_=pos2, scalar=4, op=ALU.logical_shift_right), r=poB, w=[pa])
                S.op("dve", lambda e: e.tensor_single_scalar(out=pb[:, :], in_=pos2, scalar=15, op=ALU.bitwise_and), r=poB, w=[pb])
                S.op("dve", lambda e: e.tensor_copy(out=paf[:, :], in_=pa[:, :]), r=[pa], w=[paf])
                S.op("dve", lambda e: e.tensor_copy(out=pbf[:, :], in_=pb[:, :]), r=[pb], w=[pbf])
                yield
                ix4 = ixf[:, :, :].rearrange("p (h two) k -> p h two k", two=2)
                for half, (pf, idst) in enumerate([(paf, i1), (pbf, i2)]):
                    S.op("dve", lambda e, pf=pf: e.tensor_tensor(
                        out=oh[:, :, :], in0=cst[:, IOTA0:IOTA0 + 16].unsqueeze(1).to_broadcast([128, 128, 16]),
                        in1=pf[:, :].unsqueeze(2).to_broadcast([128, 128, 16]), op=ALU.is_equal), r=[pf], w=[oh], c=[cst])
                    yield
                    S.op("dve", lambda e, half=half: e.tensor_tensor(
                        out=oh[:, :, :].rearrange("p (h k) a -> p h k a", h=8), in0=oh[:, :, :].rearrange("p (h k) a -> p h k a", h=8),
                        in1=ix4[:, :, half, :].unsqueeze(2).to_broadcast([128, 8, 16, 16]), op=ALU.mult), r=[oh, ixf], w=[oh])
                    yield
                    S.op("dve", lambda e, idst=idst: e.tensor_reduce(out=idst[:, :], in_=oh[:, :, :], axis=AX.X, op=ALU.add), r=[oh], w=[idst])
                    yield
                S.op("dve", lambda e: e.scalar_tensor_tensor(out=i1[:, :], in0=i1[:, :], scalar=128.0, in1=i2[:, :], op0=ALU.mult, op1=ALU.add),
                     r=[i1, i2], w=[i1])
                S.op("dve", lambda e: e.tensor_copy(out=ids[:, :], in_=i1[:, :]), r=[i1], w=[ids])
                yield

            def stageB(it, agen):
                seq, ti = divmod(it, ntile)
                par = it % 2
                h1, xn2, ids, gates = h1s[par], xn2s[par], idss[par], gatess[par]
                o = ot[par]
                h2 = o
                ffnb = ffnbs[par]

                def pump(n=1):
                    if agen is not None:
                        for _ in range(n):
                            try:
                                next(agen)
                            except StopIteration:
                                return

                for k in range(NSLOT):
                    uvb = UVb[k % NGB]
                    hk = hidr[k % 6]
                    d = dg[k % 6]
                    S.dma("pool", lambda e, uvb=uvb, k=k: e.indirect_dma_start(
                        out=uvb[:, :], out_offset=None, in_=uv_d[:, :],
                        in_offset=bass.IndirectOffsetOnAxis(ap=ids[:, k:k + 1], axis=0)), dst=uvb, r=[ids], c=uv_bufs)
                    jf = junkfs[k % 3]
                    S.op("dve", lambda e, uvb=uvb, hk=hk, jf=jf: e.scalar_tensor_tensor(
                        out=jf[:, :], in0=uvb[:, 0:D], scalar=1.0, in1=xn2[:, :], op0=ALU.mult, op1=ALU.mult,
                        accum_out=hk[:, 0:1]), r=[uvb, xn2], w=[hk, jf])
                    S.op("act", lambda e, hk=hk: e.activation(out=hk[:, 1:2], in_=hk[:, 0:1], func=AF.Gelu), r=[hk], w=[hk])
                    S.op("act", lambda e, hk=hk, k=k: e.activation(out=hk[:, 2:3], in_=hk[:, 1:2], func=AF.Copy, scale=gates[:, k:k + 1]),
                         r=[hk, gates], w=[hk])
                    S.op("act", lambda e, d=d, hk=hk: e.activation(out=d[:, :], in_=cst[:, 0:128], func=AF.Copy, scale=hk[:, 2:3]),
                         r=[hk], w=[d], c=[cst])
                    for nb in range(2):
                        S.op("pe", lambda e, d=d, uvb=uvb, nb=nb, k=k: e.matmul(
                            ffnb[nb][:, :], lhsT=d[:, :], rhs=uvb[:, D + nb * 512:D + (nb + 1) * 512], start=(k == 0), stop=(k == NSLOT - 1)),
                            r=[d, uvb], w=[ffnb[nb]])
                    pump(1)
                pump(100000)
                for nb in range(2):
                    S.op("dve", lambda e, nb=nb: e.tensor_tensor(out=h2[:, nb * 512:(nb + 1) * 512], in0=ffnb[nb][:, :],
                                                                 in1=h1[:, nb * 512:(nb + 1) * 512], op=ALU.add), r=[ffnb[nb], h1], w=[h2])
                S.op("act", lambda e: e.activation(out=junkb[:, :], in_=h2[:, :], func=AF.Square, accum_out=smo[:, 0:1]), r=[h2], w=[smo, junkb])
                S.op("pool", lambda e: e.tensor_scalar(out=smo[:, 1:2], in0=smo[:, 0:1], scalar1=1.0 / D, scalar2=EPS, op0=ALU.mult, op1=ALU.add),
                     r=[smo], w=[smo])
                S.op("pool", lambda e: e.tensor_tensor(out=smo[:, 2:3], in0=smo[:, 1:2], in1=mhalf[:, 0:1], op=ALU.pow), r=[smo], w=[smo], c=[mhalf])
                S.op("dve", lambda e: e.scalar_tensor_tensor(out=o[:, :], in0=h2[:, :], scalar=smo[:, 2:3], in1=bpb[:, GL0:GL0 + D],
                                                             op0=ALU.mult, op1=ALU.mult), r=[h2, smo], w=[o], c=[bpb])
                S.dma("sp", lambda e: e.dma_start(out=out_d[seq, ti * 128:(ti + 1) * 128, :], in_=o[:, :]), dst=out_b, r=[o])

            ntot = nseq * ntile
            g0 = stageA(0)
            for _ in g0:
                pass
            for it in range(ntot):
                agen = stageA(it + 1) if it + 1 < ntot else None
                stageB(it, agen)
            S.final_wait("sp", [out_b])
    return nc


def _host_inputs(inputs):
    f = lambda a: np.ascontiguousarray(np.asarray(a, dtype=np.float32))
    x = f(inputs["x"])
    pp = np.concatenate([
        f(inputs["g_mix"])[0].reshape(8, 128).T,
        f(inputs["conv_ssd_w"])[0].T.reshape(20, 128, 4).transpose(1, 0, 2).reshape(128, 80),
        f(inputs["conv_ssd_b"])[0].reshape(20, 128).T,
        f(inputs["conv_sc_w"])[0].T.reshape(4, 128, 3).transpose(1, 0, 2).reshape(128, 12),
        np.concatenate([f(inputs["ssd_norm_w"])[0], np.ones(512, np.float32)]).reshape(16, 128).T,
    ], axis=1)
    bpa = np.concatenate([f(inputs["dt_bias"])[0], f(inputs["a_log"])[0], f(inputs["d_skip"])[0]])[None, :]
    bpb = np.concatenate([f(inputs["g_ffn"])[0], f(inputs["g_final"])])[None, :]
    skt = f(inputs["sub_keys"])[0].transpose(3, 0, 1, 2).reshape(128, 2048)
    r = np.arange(128)
    cst = np.concatenate([
        np.eye(128, dtype=np.float32),
        (r[:, None] <= r[None, :]).astype(np.float32),
        np.ones((128, 128), np.float32),
        np.where(r[None, :] < r[:, None], np.float32(NEG), np.float32(0.0)).astype(np.float32),
        np.broadcast_to(np.arange(16, dtype=np.float32)[None, :], (128, 16)),
    ], axis=1)
    shared = {
        "meta": f(inputs["meta_tokens"]), "w_in": f(inputs["w_in"])[0], "w_out": f(inputs["w_out"])[0],
        "w_q": f(inputs["w_q"])[0], "skt": np.ascontiguousarray(skt), "expert_u": f(inputs["expert_u"])[0],
        "expert_v": f(inputs["expert_v"])[0], "pp": np.ascontiguousarray(pp), "bpa": np.ascontiguousarray(bpa),
        "bpb": np.ascontiguousarray(bpb), "cst": np.ascontiguousarray(cst),
    }
    return x, shared


def kernel(**inputs):
    x, shared = _host_inputs(inputs)
    ncores = 8
    nc = build()
    in_maps = []
    for c in range(ncores):
        m = dict(shared)
        m["x"] = np.ascontiguousarray(x[c * NSEQ:(c + 1) * NSEQ])
        in_maps.append(m)
    res = run_bass_kernel_spmd(nc, in_maps, core_ids=list(range(ncores)))
    out = np.concatenate([np.asarray(r["out"], dtype=np.float32) for r in res.results], axis=0)
    return out
```
